# Optimizing a Trainium2 kernel written in Bass

```python
import jax, jax.numpy as jnp
from jax import lax
import numpy as np

D_MODEL = 1024
BATCH = 16
SEQ = 2048
DEPTH = 2

HEAD_DIM = 64
SWA_HEADS = D_MODEL // 128
SWA_KV_HEADS = 2
SWA_GROUP = SWA_HEADS // SWA_KV_HEADS
WINDOW = 128
BLOCK = 128
SB_HEADS = D_MODEL // 128
LRU_WIDTH = D_MODEL // 2
LRU_HEADS = 8
LRU_BLK = LRU_WIDTH // LRU_HEADS
LRU_C = 8.0
CONV_W = 4
N_BRANCHES = 3
D_FF = 7 * D_MODEL // 2
N_EXPERTS = 8
TOP_K = 2
N_DENSE = (DEPTH + 1) // 2
N_MOE = DEPTH // 2
EPS = 1e-6

SWA_Q = SWA_HEADS * HEAD_DIM
SWA_KV = SWA_KV_HEADS * HEAD_DIM
SB_W = SB_HEADS * HEAD_DIM
IN_WIDTHS = (SWA_Q, SWA_KV, SWA_KV, SB_W, SB_W, SB_W, LRU_WIDTH, LRU_WIDTH, N_BRANCHES * D_MODEL)
IN_COLS = sum(IN_WIDTHS)
IN_SPLITS = tuple(int(v) for v in np.cumsum(IN_WIDTHS)[:-1])

kernel_name = "hybrid_swa_stickbreak_rglru_moe"


def rms_norm(x, g):
    xf = x.astype(jnp.float32)
    y = xf * lax.rsqrt(jnp.mean(xf * xf, axis=-1, keepdims=True) + EPS)
    return (y * g.astype(jnp.float32)).astype(x.dtype)


def sliding_window_attention(q, k, v, sinks):
    B, S = q.shape[:2]
    nb = S // BLOCK
    qb = q.reshape(B, nb, BLOCK, SWA_KV_HEADS, SWA_GROUP, HEAD_DIM)

    def band(t):
        tb = t.reshape(B, nb, BLOCK, SWA_KV_HEADS, HEAD_DIM)
        prev = jnp.pad(tb, ((0, 0), (1, 0), (0, 0), (0, 0), (0, 0)))[:, :-1]
        return jnp.concatenate([prev, tb], axis=2)

    kb, vb = band(k), band(v)
    scores = jnp.einsum('bnqhgd,bnkhd->bnhgqk', qb, kb).astype(jnp.float32) * (HEAD_DIM ** -0.5)
    qpos = jnp.arange(BLOCK)[:, None] + BLOCK
    kpos = jnp.arange(2 * BLOCK)[None, :]
    rel = qpos - kpos
    in_window = (rel >= 0) & (rel < WINDOW)
    in_range = (jnp.arange(nb)[:, None, None] > 0) | (kpos[None] >= BLOCK)
    mask = in_window[None] & in_range
    scores = jnp.where(mask[None, :, None, None], scores, -jnp.inf)
    sink = jnp.broadcast_to(
        sinks.astype(jnp.float32).reshape(1, 1, SWA_KV_HEADS, SWA_GROUP, 1, 1),
        scores.shape[:-1] + (1,))
    probs = jax.nn.softmax(jnp.concatenate([scores, sink], axis=-1), axis=-1)[..., :-1]
    out = jnp.einsum('bnhgqk,bnkhd->bnqhgd', probs.astype(v.dtype), vb)
    return out.reshape(B, S, SWA_Q)


def stick_breaking_attention(q, k, v):
    B, S = q.shape[:2]
    scale = HEAD_DIM ** -0.5
    outs = []
    for blk in range(S // BLOCK):
        start, end = blk * BLOCK, (blk + 1) * BLOCK
        z = jnp.einsum('bqhd,bkhd->bhqk', q[:, start:end], k[:, :end]).astype(jnp.float32) * scale
        tpos = start + jnp.arange(BLOCK)[:, None]
        spos = jnp.arange(end)[None, :]
        strict = spos < tpos
        log_fail = jnp.where(strict, jax.nn.log_sigmoid(-z), 0.0)
        later = lax.cumsum(log_fail, axis=log_fail.ndim - 1, reverse=True) - log_fail
        weights = jnp.where(strict, jnp.exp(jax.nn.log_sigmoid(z) + later), 0.0)
        outs.append(jnp.einsum('bhqk,bkhd->bqhd', weights.astype(v.dtype), v[:, :end]))
    return jnp.concatenate(outs, axis=1).reshape(B, S, SB_W)


def causal_depthwise_conv(x, w, b):
    S = x.shape[1]
    xp = jnp.pad(x, ((0, 0), (CONV_W - 1, 0), (0, 0)))
    y = xp[:, 0:S] * w[0]
    for tap in range(1, CONV_W):
        y = y + xp[:, tap:tap + S] * w[tap]
    return y + b


def rg_lru(x, w_r, b_r, w_i, b_i, lam):
    B, S, W = x.shape
    xh = x.reshape(B, S, LRU_HEADS, LRU_BLK)
    r = jax.nn.sigmoid((jnp.einsum('bshi,hij->bshj', xh, w_r).reshape(B, S, W) + b_r).astype(jnp.float32))
    i = jax.nn.sigmoid((jnp.einsum('bshi,hij->bshj', xh, w_i).reshape(B, S, W) + b_i).astype(jnp.float32))
    log_a = -LRU_C * r * jax.nn.softplus(-lam.astype(jnp.float32))
    a = jnp.exp(log_a)
    u = jnp.sqrt(-jnp.expm1(2.0 * log_a)) * (i * x.astype(jnp.float32))

    def combine(left, right):
        a1, b1 = left
        a2, b2 = right
        return a1 * a2, a2 * b1 + b2

    _, h = lax.associative_scan(combine, (a, u), axis=1)
    return h.astype(x.dtype)


def hybrid_mixer(h, w_in, b_gate, q_norm, k_norm, sinks, conv_w, conv_b,
                 lru_w_r, lru_b_r, lru_w_i, lru_b_i, lru_lambda,
                 w_proj_a, w_proj_b, w_proj_c, w_out):
    B, S, _ = h.shape
    proj = h @ w_in
    qa, ka, va, qb, kb, vb, xc, gc, gates = jnp.split(proj, IN_SPLITS, axis=-1)

    qa = rms_norm(qa.reshape(B, S, SWA_HEADS, HEAD_DIM), q_norm)
    ka = rms_norm(ka.reshape(B, S, SWA_KV_HEADS, HEAD_DIM), k_norm)
    va = va.reshape(B, S, SWA_KV_HEADS, HEAD_DIM)
    o_a = sliding_window_attention(qa, ka, va, sinks)

    o_b = stick_breaking_attention(qb.reshape(B, S, SB_HEADS, HEAD_DIM),
                                   kb.reshape(B, S, SB_HEADS, HEAD_DIM),
                                   vb.reshape(B, S, SB_HEADS, HEAD_DIM))

    xc = causal_depthwise_conv(xc, conv_w, conv_b)
    o_c = rg_lru(xc, lru_w_r, lru_b_r, lru_w_i, lru_b_i, lru_lambda) * jax.nn.gelu(gc)

    g = jax.nn.sigmoid(gates + b_gate).reshape(B, S, N_BRANCHES, D_MODEL)
    merged = (g[:, :, 0] * (o_a @ w_proj_a)
              + g[:, :, 1] * (o_b @ w_proj_b)
              + g[:, :, 2] * (o_c @ w_proj_c))
    return merged @ w_out


def swiglu(h, w_gate, w_up, w_down):
    return (jax.nn.silu(h @ w_gate) * (h @ w_up)) @ w_down


def moe_swiglu(h, w_router, w_gate, w_up, w_down):
    B, S, D = h.shape
    hf = h.reshape(B * S, D)
    logits = (hf @ w_router).astype(jnp.float32)
    top_logits, top_idx = lax.top_k(logits, TOP_K)
    top_w = jax.nn.softmax(top_logits, axis=-1)
    combine = jnp.einsum('nk,nke->ne', top_w,
                         jax.nn.one_hot(top_idx, N_EXPERTS, dtype=jnp.float32)).astype(h.dtype)
    out = jnp.zeros_like(hf)
    for e in range(N_EXPERTS):
        out = out + combine[:, e:e + 1] * swiglu(hf, w_gate[e], w_up[e], w_down[e])
    return out.reshape(B, S, D)


def setup_inputs(seed: int = 0) -> dict:
    key = jax.random.key(seed)
    ks = jax.random.split(key, 26)
    f32 = jnp.float32

    def nrm(k, shape, scale):
        return jax.random.normal(k, shape, f32) * scale

    u = jax.random.uniform(ks[13], (DEPTH, LRU_WIDTH), f32, minval=0.9, maxval=0.999)
    p = u ** (1.0 / LRU_C)
    lru_lambda = jnp.log(p) - jnp.log1p(-p)
    return {
        "x": nrm(ks[0], (BATCH, SEQ, D_MODEL), 1.0),
        "attn_norm": 1.0 + nrm(ks[1], (DEPTH, D_MODEL), 0.02),
        "w_in": nrm(ks[2], (DEPTH, D_MODEL, IN_COLS), D_MODEL ** -0.5),
        "b_gate": nrm(ks[3], (DEPTH, N_BRANCHES * D_MODEL), 0.02),
        "q_norm": 1.0 + nrm(ks[4], (DEPTH, HEAD_DIM), 0.02),
        "k_norm": 1.0 + nrm(ks[5], (DEPTH, HEAD_DIM), 0.02),
        "sinks": nrm(ks[6], (DEPTH, SWA_HEADS), 1.0),
        "conv_w": nrm(ks[7], (DEPTH, CONV_W, LRU_WIDTH), CONV_W ** -0.5),
        "conv_b": nrm(ks[8], (DEPTH, LRU_WIDTH), 0.02),
        "lru_w_r": nrm(ks[9], (DEPTH, LRU_HEADS, LRU_BLK, LRU_BLK), LRU_BLK ** -0.5),
        "lru_b_r": nrm(ks[10], (DEPTH, LRU_WIDTH), 0.02),
        "lru_w_i": nrm(ks[11], (DEPTH, LRU_HEADS, LRU_BLK, LRU_BLK), LRU_BLK ** -0.5),
        "lru_b_i": nrm(ks[12], (DEPTH, LRU_WIDTH), 0.02),
        "lru_lambda": lru_lambda,
        "w_proj_a": nrm(ks[14], (DEPTH, SWA_Q, D_MODEL), SWA_Q ** -0.5),
        "w_proj_b": nrm(ks[15], (DEPTH, SB_W, D_MODEL), SB_W ** -0.5),
        "w_proj_c": nrm(ks[16], (DEPTH, LRU_WIDTH, D_MODEL), LRU_WIDTH ** -0.5),
        "w_out": nrm(ks[17], (DEPTH, D_MODEL, D_MODEL), D_MODEL ** -0.5),
        "ffn_norm": 1.0 + nrm(ks[18], (DEPTH, D_MODEL), 0.02),
        "w_ffn_gate": nrm(ks[19], (N_DENSE, D_MODEL, D_FF), D_MODEL ** -0.5),
        "w_ffn_up": nrm(ks[20], (N_DENSE, D_MODEL, D_FF), D_MODEL ** -0.5),
        "w_ffn_down": nrm(ks[21], (N_DENSE, D_FF, D_MODEL), D_FF ** -0.5),
        "w_router": nrm(ks[22], (N_MOE, D_MODEL, N_EXPERTS), D_MODEL ** -0.5),
        "w_exp_gate": nrm(ks[23], (N_MOE, N_EXPERTS, D_MODEL, D_FF), D_MODEL ** -0.5),
        "w_exp_up": nrm(ks[24], (N_MOE, N_EXPERTS, D_MODEL, D_FF), D_MODEL ** -0.5),
        "w_exp_down": nrm(ks[25], (N_MOE, N_EXPERTS, D_FF, D_MODEL), D_FF ** -0.5),
    }


def reference(x, attn_norm, w_in, b_gate, q_norm, k_norm, sinks, conv_w, conv_b,
              lru_w_r, lru_b_r, lru_w_i, lru_b_i, lru_lambda,
              w_proj_a, w_proj_b, w_proj_c, w_out, ffn_norm,
              w_ffn_gate, w_ffn_up, w_ffn_down,
              w_router, w_exp_gate, w_exp_up, w_exp_down):
    for l in range(DEPTH):
        h = rms_norm(x, attn_norm[l])
        x = x + hybrid_mixer(h, w_in[l], b_gate[l], q_norm[l], k_norm[l], sinks[l],
                             conv_w[l], conv_b[l], lru_w_r[l], lru_b_r[l],
                             lru_w_i[l], lru_b_i[l], lru_lambda[l],
                             w_proj_a[l], w_proj_b[l], w_proj_c[l], w_out[l])
        h = rms_norm(x, ffn_norm[l])
        if l % 2 == 0:
            j = l // 2
            x = x + swiglu(h, w_ffn_gate[j], w_ffn_up[j], w_ffn_down[j])
        else:
            j = l // 2
            x = x + moe_swiglu(h, w_router[j], w_exp_gate[j], w_exp_up[j], w_exp_down[j])
    return x
```

```python
import numpy as np
import concourse.bass as bass
import concourse.mybir as mybir
from concourse.bass_utils import run_bass_kernel_spmd

F32 = mybir.dt.float32
BF16 = mybir.dt.bfloat16
AF = mybir.ActivationFunctionType
ALU = mybir.AluOpType
AX = mybir.AxisListType

D = 1024
HD = 64
INC = 6400
DFF = 3584
NE = 8
EPS = 1e-6
O_QA, O_KA, O_VA, O_QB, O_KB, O_VB, O_XC, O_GC, O_GT = 0, 512, 640, 768, 1280, 1792, 2304, 2816, 3328
NCORES = 8


class Buf:
    __slots__ = ("name", "w", "rs")

    def __init__(self, name):
        self.name = name
        self.w = None
        self.rs = {}


class Sched:
    SEM_LIMIT = 30000
    NDMA = 6

    def __init__(self, nc):
        self.nc = nc
        self.names = ["pe", "act", "dve", "pool", "sp"]
        self.lists = {k: [] for k in self.names}
        self.seen = {k: {} for k in self.names}
        self.cur = {}
        self.sems = {}
        self.last = {}
        self.dma_slots = {}
        self.dma_rr = {k: 0 for k in self.names}
        self.ctxs = []

    def _newsem(self, name):
        cm = self.nc.semaphore(name)
        self.sems[name] = cm.__enter__()
        self.ctxs.append(cm)
        return name

    def _eng_event(self, eng):
        c = self.cur.get(eng)
        if c is None or c[1] >= self.SEM_LIMIT:
            ep = 0 if c is None else c[2] + 1
            c = [self._newsem(f"s_{eng}_{ep}"), 0, ep]
            self.cur[eng] = c
        c[1] += 1
        self.last[c[0]] = c[1]
        return (c[0], c[1])

    def _need(self, eng, evs):
        out = {}
        for ev in evs:
            if ev is None:
                continue
            k, v = ev
            if self.seen[eng].get(k, 0) >= v:
                continue
            if out.get(k, 0) < v:
                out[k] = v
        for k, v in out.items():
            self.seen[eng][k] = v
        return list(out.items())

    def _deps(self, eng, reads, writes):
        c = self.cur.get(eng)
        own = c[0] if c else None
        evs = []
        for b in reads:
            evs.append(b.w)
        for b in writes:
            evs.append(b.w)
            evs.extend(b.rs.items())
        if eng == "pe":
            evs = [e for e in evs if e is not None and e[0] != own]
        return self._need(eng, evs)

    def _commit(self, ev, reads, writes):
        for b in reads:
            if b.rs.get(ev[0], 0) < ev[1]:
                b.rs[ev[0]] = ev[1]
        for b in writes:
            b.w = ev
            b.rs = {}

    def op(self, eng, fn, reads=(), writes=()):
        waits = self._deps(eng, reads, writes)
        ev = self._eng_event(eng)
        self.lists[eng].append((waits, fn, ev, 1))
        self._commit(ev, reads, writes)
        return ev

    def dma(self, q, out, in_, reads=(), writes=(), **kw):
        slots = self.dma_slots.setdefault(q, [])
        if len(slots) < self.NDMA:
            slots.append([self._newsem(f"d_{q}_{len(slots)}"), 0])
        slot = slots[self.dma_rr[q] % self.NDMA]
        self.dma_rr[q] += 1
        waits = self._deps(q, reads, writes)
        if slot[1] > 0:
            waits += self._need(q, [(slot[0], slot[1])])
        slot[1] += 16
        ev = (slot[0], slot[1])
        self.last[slot[0]] = slot[1]
        fn = lambda e: e.dma_start(out=out, in_=in_, **kw)
        self.lists[q].append((waits, fn, ev, 16))
        self._commit(ev, reads, writes)
        return ev

    def barrier(self):
        evs = list(self.last.items())
        for eng in self.names:
            waits = self._need(eng, evs)
            if waits:
                self.lists[eng].append((waits, None, None, 0))

    def emit(self):
        nc = self.nc
        engs = {"pe": "tensor", "act": "scalar", "dve": "vector", "pool": "gpsimd", "sp": "sync"}
        with nc.Block() as block:
            def mk(name):
                def body(e):
                    for waits, fn, ev, inc in self.lists[name]:
                        for k, v in waits:
                            e.wait_ge(self.sems[k], v)
                        if fn is not None:
                            fn(e).then_inc(self.sems[ev[0]], inc)
                return body
            for name in self.names:
                getattr(block, engs[name])(mk(name))
        for cm in reversed(self.ctxs):
            cm.__exit__(None, None, None)


def build(S=2048, NSEQ=2, DEPTH=2, dbg=None):
    NB = S // 128
    NG = S // 512
    LT = 512
    nc = bass.Bass("TRN2", target_bir_lowering=False)
    din = {}

    def dram_in(name, shape):
        din[name] = nc.dram_tensor(name, list(shape), F32, kind="ExternalInput").ap()

    NDENSE = (DEPTH + 1) // 2
    NMOE = max(DEPTH // 2, 1)
    dram_in("x", [NSEQ, S, D])
    dram_in("attn_norm", [DEPTH, D]); dram_in("w_in", [DEPTH, D, INC]); dram_in("b_gate", [DEPTH, 3 * D])
    dram_in("q_norm", [DEPTH, HD]); dram_in("k_norm", [DEPTH, HD]); dram_in("sinks", [DEPTH, 8])
    dram_in("conv_w", [DEPTH, 4, 512]); dram_in("conv_b", [DEPTH, 512])
    dram_in("lru_w_r", [DEPTH, 8, 64, 64]); dram_in("lru_b_r", [DEPTH, 512])
    dram_in("lru_w_i", [DEPTH, 8, 64, 64]); dram_in("lru_b_i", [DEPTH, 512]); dram_in("lru_lambda", [DEPTH, 512])
    dram_in("w_proj_a", [DEPTH, 512, D]); dram_in("w_proj_b", [DEPTH, 512, D]); dram_in("w_proj_c", [DEPTH, 512, D])
    dram_in("w_out", [DEPTH, D, D]); dram_in("ffn_norm", [DEPTH, D])
    dram_in("w_ffn_gate", [NDENSE, D, DFF]); dram_in("w_ffn_up", [NDENSE, D, DFF]); dram_in("w_ffn_down", [NDENSE, DFF, D])
    dram_in("w_router", [NMOE, D, NE]); dram_in("w_exp_gate", [NMOE, NE, D, DFF])
    dram_in("w_exp_up", [NMOE, NE, D, DFF]); dram_in("w_exp_down", [NMOE, NE, DFF, D])
    out = nc.dram_tensor("out", [NSEQ, S, D], F32, kind="ExternalOutput").ap()

    S_ = Sched(nc)
    ARENA = 208000
    arena = nc.alloc_sbuf_tensor("arena", [128, ARENA // 4], F32).ap()
    psum = nc.alloc_psum_tensor("psum", [128, 8, 512], F32).ap()

    def bank(i):
        return psum[:, i, :]

    def view(off, shape, dt):
        n = int(np.prod(shape[1:]))
        sz = 4 if dt == F32 else 2
        assert off % 4 == 0 and (n * sz) % 4 == 0, (off, shape)
        assert off + n * sz <= ARENA, ("arena overflow", off, shape)
        w = arena[:, off // 4:(off + n * sz) // 4]
        v = w if dt == F32 else w.bitcast(dt)
        if len(shape) == 3:
            v = v.rearrange("p (a b) -> p a b", b=shape[2])
        elif len(shape) == 4:
            v = v.rearrange("p (a b c) -> p a b c", b=shape[2], c=shape[3])
        return v

    class Alloc:
        def __init__(self, base):
            self.off = base

        def __call__(self, shape, dt):
            n = int(np.prod(shape[1:])) * (4 if dt == F32 else 2)
            n = (n + 31) // 32 * 32
            v = view(self.off, shape, dt)
            self.off += n
            return v

    pa = Alloc(0)
    identb = pa([128, 128], BF16)
    identf = pa([128, 128], F32)
    onesb = pa([128, 128], BF16)
    Uneg = pa([128, 128], BF16)
    onesneg = pa([128, 128], BF16)
    mD = pa([128, 512], BF16)
    mP = pa([128, 512], BF16)
    sbm = pa([128, 4, 512], BF16)
    gt = pa([128, D], F32)
    qg8 = pa([128, HD], F32)
    kg = pa([128, HD], F32)
    esk = pa([128, 8], F32)
    cw = pa([128, 4, 4], F32)
    cb = pa([128, 4], F32)
    br = pa([128, 4], F32)
    bi = pa([128, 4], F32)
    clam = pa([128, 4], F32)
    bg = pa([128, 24], F32)
    wbd = pa([128, 4, 2, 128], BF16)
    comb = pa([128, NB, NE], F32)
    hprev = pa([128, 2], F32)
    xres = pa([128, NB, D], F32)
    hT = pa([128, 8, S], BF16)
    O_BASE = pa.off
    oT = [pa([128, 4, S], BF16) for _ in range(3)]
    W_BASE = pa.off

    B = {}

    def b(name):
        if name not in B:
            B[name] = Buf(name)
        return B[name]

    PB = [b(f"bank{i}") for i in range(8)]

    def MM(out, lhsT, rhs, start, stop, reads, writes, skip=False):
        S_.op("pe", lambda e: e.matmul(out=out, lhsT=lhsT, rhs=rhs, start=start, stop=stop, skip_group_check=skip), reads, writes)

    def TR(out, in_, ident, reads, writes):
        S_.op("pe", lambda e: e.transpose(out=out, in_=in_, identity=ident), reads, writes)

    def ACT(out, in_, func, reads, writes, **kw):
        S_.op("act", lambda e: e.activation(out=out, in_=in_, func=func, **kw), reads, writes)

    def TT(eng, out, in0, in1, op, reads, writes):
        S_.op(eng, lambda e: e.tensor_tensor(out=out, in0=in0, in1=in1, op=op), reads, writes)

    def TS(eng, out, in0, s1, s2, op0, op1, reads, writes):
        S_.op(eng, lambda e: e.tensor_scalar(out=out, in0=in0, scalar1=s1, scalar2=s2, op0=op0, op1=op1), reads, writes)

    def STT(out, in0, scalar, in1, op0, op1, reads, writes):
        S_.op("dve", lambda e: e.scalar_tensor_tensor(out=out, in0=in0, scalar=scalar, in1=in1, op0=op0, op1=op1), reads, writes)

    def CP(eng, out, in_, reads, writes):
        S_.op(eng, lambda e: e.tensor_copy(out=out, in_=in_), reads, writes)

    def MS(eng, ap, val, writes):
        S_.op(eng, lambda e: e.memset(ap, val), (), writes)

    def ASEL(out, in_, pattern, cmp, fill, base, cm, reads, writes):
        S_.op("pool", lambda e: e.affine_select(out=out, in_=in_, pattern=pattern, compare_op=cmp, fill=fill,
                                                base=base, channel_multiplier=cm), reads, writes)

    def LD(q, out_, in_, writes, **kw):
        S_.dma(q, out_, in_, (), writes, **kw)

    bank7b = psum[:, 7, :].bitcast(BF16)

    tmpc = view(W_BASE, [128, 512], F32)
    cB = b("consts")
    MS("pool", identf, 0.0, [cB])
    ASEL(identf, identf, [[-1, 128]], ALU.not_equal, 1.0, 0, 1, [cB], [cB])
    CP("dve", identb, identf, [cB], [cB])
    MS("pool", onesb, 1.0, [cB])
    MS("pool", onesneg, -1.0, [cB])
    MS("pool", tmpc[:, 0:128], -1.0, [cB])
    ASEL(tmpc[:, 0:128], tmpc[:, 0:128], [[-1, 128]], ALU.is_ge, 0.0, 0, 1, [cB], [cB])
    CP("dve", Uneg, tmpc[:, 0:128], [cB], [cB])
    MS("pool", tmpc, 1.0, [cB])
    ASEL(tmpc.rearrange("p (a t) -> p a t", t=128), tmpc.rearrange("p (a t) -> p a t", t=128), [[0, 4], [1, 128]],
         ALU.is_ge, 0.0, 0, -1, [cB], [cB])
    TS("dve", tmpc, tmpc, 30000.0, -30000.0, ALU.mult, ALU.add, [cB], [cB])
    CP("dve", mD, tmpc, [cB], [cB])
    MS("pool", tmpc, 1.0, [cB])
    ASEL(tmpc.rearrange("p (a t) -> p a t", t=128), tmpc.rearrange("p (a t) -> p a t", t=128), [[0, 4], [-1, 128]],
         ALU.is_gt, 0.0, 0, 1, [cB], [cB])
    TS("dve", tmpc, tmpc, 30000.0, -30000.0, ALU.mult, ALU.add, [cB], [cB])
    CP("dve", mP, tmpc, [cB], [cB])
    for k in range(4):
        MS("pool", tmpc, 1.0, [cB])
        ASEL(tmpc, tmpc, [[1, 512]], ALU.is_gt, 0.0, -128 * k, -1, [cB], [cB])
        TS("dve", tmpc, tmpc, 30000.0, -30000.0, ALU.mult, ALU.add, [cB], [cB])
        CP("dve", sbm[:, k, :], tmpc, [cB], [cB])
    S_.barrier()

    XB = [[b(f"x{tb}_{cg}") for cg in range(2)] for tb in range(NB)]
    HB = b("hT")
    OB = [b(f"oT{i}") for i in range(3)]

    def xbufs(tb):
        return XB[tb]

    SMALL = [b(n) for n in ("s_qg8", "s_kg", "s_esk", "s_cw0", "s_cw1", "s_cw2", "s_cw3", "s_cb", "s_br", "s_bi", "s_clam", "s_bg")]
    WBD = [b(f"s_wbd{i}") for i in range(16)]

    def load_layer_consts(l):
        NCD = dict(allow_slow_non_contiguous=True)
        sq_, sk_, se_, c0, c1, c2, c3, scb, sbr, sbi, scl, sbg = SMALL
        LD("sp", qg8, din["q_norm"][l].partition_broadcast(128), [sq_])
        LD("sp", kg, din["k_norm"][l].partition_broadcast(128), [sk_])
        LD("sp", esk, din["sinks"][l].partition_broadcast(128), [se_])
        for k, cB_ in enumerate((c0, c1, c2, c3)):
            LD("sp", cw[:, :, k], din["conv_w"][l, k].rearrange("(c p) -> p c", p=128), [cB_], **NCD)
        LD("sp", cb, din["conv_b"][l].rearrange("(c p) -> p c", p=128), [scb], **NCD)
        LD("sp", br, din["lru_b_r"][l].rearrange("(c p) -> p c", p=128), [sbr], **NCD)
        LD("sp", bi, din["lru_b_i"][l].rearrange("(c p) -> p c", p=128), [sbi], **NCD)
        LD("sp", clam, din["lru_lambda"][l].rearrange("(c p) -> p c", p=128), [scl], **NCD)
        LD("sp", bg, din["b_gate"][l].rearrange("(c p) -> p c", p=128), [sbg], **NCD)
        MS("pool", wbd, 0.0, WBD)
        n_ = 0
        for c in range(4):
            for hh in range(2):
                LD("pool", wbd[hh * 64:(hh + 1) * 64, c, 0, hh * 64:(hh + 1) * 64], din["lru_w_r"][l, 2 * c + hh], [WBD[n_]])
                LD("pool", wbd[hh * 64:(hh + 1) * 64, c, 1, hh * 64:(hh + 1) * 64], din["lru_w_i"][l, 2 * c + hh], [WBD[n_ + 1]])
                n_ += 2
        TS("dve", qg8, qg8, 0.125, None, ALU.mult, ALU.bypass, [sq_], [sq_])
        ACT(esk, esk, AF.Exp, [se_], [se_])
        ACT(clam, clam, AF.Exp, [scl], [scl], scale=-1.0)
        ACT(clam, clam, AF.Ln, [scl], [scl], bias=1.0)
        TS("dve", clam, clam, -8.0, None, ALU.mult, ALU.bypass, [scl], [scl])
        S_.barrier()

    def norm_phase(gain_dram, base, router_w=None):
        wa = Alloc(base)
        junk = wa([128, D], BF16)
        hb = [wa([128, D], BF16) for _ in range(2)]
        ss = wa([128, 4], F32)
        gB = b("gt")
        LD("sp", gt, gain_dram.partition_broadcast(128), [gB])
        if router_w is not None:
            hf = wa([128, D], F32)
            hTf = wa([128, 8, 128], F32)
            wr = wa([128, 8, NE], F32)
            lg = wa([128, NE], F32)
            t8 = [wa([128, NE], F32) for _ in range(3)]
            m12 = wa([128, 4], F32)
            LD("sp", wr, router_w.rearrange("(kc p) e -> p kc e", p=128), [b("wr")])
        for tb in range(NB):
            par = tb % 2
            xr = xres[:, tb, :]
            ssB, hbB = b(f"ss{par}"), b(f"hb{par}")
            ACT(junk, xr, AF.Square, xbufs(tb), [b("junk"), ssB], accum_out=ss[:, par:par + 1])
            ACT(ss[:, par:par + 1], ss[:, par:par + 1], AF.Ln, [ssB], [ssB], scale=1.0 / D, bias=EPS)
            ACT(ss[:, par:par + 1], ss[:, par:par + 1], AF.Exp, [ssB], [ssB], scale=-0.5)
            STT(hb[par], xr, ss[:, par:par + 1], gt, ALU.mult, ALU.mult, xbufs(tb) + [ssB, gB], [hbB])
            tbk = 7 if par == 0 else 3
            tbv = psum[:, tbk, :].bitcast(BF16)
            for c in range(8):
                TR(tbv[:, c * 128:(c + 1) * 128], hb[par][:, c * 128:(c + 1) * 128], identb, [hbB], [PB[tbk]])
            if tb % 2:
                CP("dve", hT[:, :, tb * 128:(tb + 1) * 128], tbv.rearrange("p (c t) -> p c t", t=128), [PB[tbk]], [HB])
            else:
                S_.op("act", lambda e, tb=tb, tbv=tbv: e.copy(out=hT[:, :, tb * 128:(tb + 1) * 128],
                                                              in_=tbv.rearrange("p (c t) -> p c t", t=128)), [PB[tbk]], [HB])
            if router_w is not None:
                STT(hf, xr, ss[:, par:par + 1], gt, ALU.mult, ALU.mult, xbufs(tb) + [ssB, gB], [b("hf")])
                for c in range(8):
                    TR(psum[:, 5 + c // 4, (c % 4) * 128:(c % 4 + 1) * 128], hf[:, c * 128:(c + 1) * 128], identf,
                       [b("hf")], [PB[5 + c // 4]])
                CP("dve", hTf, psum[:, 5:7, :].rearrange("p a (c t) -> p (a c) t", t=128), [PB[5], PB[6]], [b("hTf")])
                for c in range(8):
                    MM(psum[:, 4, 0:NE], hTf[:, c, :], wr[:, c, :], c == 0, c == 7, [b("hTf"), b("wr")], [PB[4]])
                lB = b("lg")
                CP("dve", lg, psum[:, 4, 0:NE], [PB[4]], [lB])
                S_.op("dve", lambda e: e.reduce_max(out=m12[:, 0:1], in_=lg, axis=AX.X), [lB], [b("m12")])
                TS("dve", t8[0], lg, m12[:, 0:1], None, ALU.is_equal, ALU.bypass, [lB, b("m12")], [b("t80")])
                STT(t8[1], t8[0], -1e30, lg, ALU.mult, ALU.add, [b("t80"), lB], [b("t81")])
                S_.op("dve", lambda e: e.reduce_max(out=m12[:, 1:2], in_=t8[1], axis=AX.X), [b("t81")], [b("m12")])
                TS("dve", t8[0], lg, m12[:, 1:2], None, ALU.is_ge, ALU.bypass, [lB, b("m12")], [b("t80")])
                TS("dve", m12[:, 2:3], m12[:, 0:1], -1.0, None, ALU.mult, ALU.bypass, [b("m12")], [b("m12")])
                ACT(t8[1], lg, AF.Exp, [lB, b("m12")], [b("t81")], bias=m12[:, 2:3], scale=1.0)
                TT("dve", t8[2], t8[1], t8[0], ALU.mult, [b("t81"), b("t80")], [b("t82")])
                S_.op("dve", lambda e: e.reduce_sum(out=m12[:, 3:4], in_=t8[2], axis=AX.X), [b("t82")], [b("m12")])
                S_.op("dve", lambda e: e.reciprocal(out=m12[:, 3:4], in_=m12[:, 3:4]), [b("m12")], [b("m12")])
                TS("dve", comb[:, tb, :], t8[2], m12[:, 3:4], None, ALU.mult, ALU.bypass, [b("t82"), b("m12")], [b("comb")])
        S_.barrier()

    def lru_phase(l):
        wa = Alloc(W_BASE)
        xraw = wa([128, S + 4], F32)
        gg = wa([128, S], BF16)
        y2 = [wa([128, LT], F32) for _ in range(2)]
        r2 = [wa([128, LT], F32) for _ in range(2)]
        ii2 = [wa([128, LT], F32) for _ in range(2)]
        t12 = [wa([128, LT], F32) for _ in range(2)]
        xcb2 = [wa([128, LT], BF16) for _ in range(2)]
        ti_ = 0
        wl = [wa([128, 8, 2, 128], BF16) for _ in range(2)]
        win = din["w_in"][l]
        MS("pool", xraw[:, 0:3], 0.0, [b("xraw")])
        bi_ = 0
        def load_wl(c):
            LD("pool", wl[c % 2][:, :, 0, :], win[:, O_XC + c * 128:O_XC + (c + 1) * 128].rearrange("(kc p) n -> p kc n", p=128), [b(f"wl{c % 2}")])
            LD("pool", wl[c % 2][:, :, 1, :], win[:, O_GC + c * 128:O_GC + (c + 1) * 128].rearrange("(kc p) n -> p kc n", p=128), [b(f"wl{c % 2}")])

        load_wl(0)
        for c in range(4):
            wB = b(f"wl{c % 2}")
            for tg in range(NG):
                for which in range(2):
                    bk = bi_ % 4
                    bi_ += 1
                    for kc in range(8):
                        MM(bank(bk), wl[c % 2][:, kc, which, :], hT[:, kc, tg * 512:(tg + 1) * 512], kc == 0, kc == 7, [wB, HB], [PB[bk]])
                    if which == 0:
                        S_.op("act", lambda e, bk=bk, tg=tg: e.copy(out=xraw[:, 3 + tg * 512:3 + (tg + 1) * 512], in_=bank(bk)), [PB[bk]], [b("xraw")])
                    else:
                        ACT(gg[:, tg * 512:(tg + 1) * 512], bank(bk), AF.Gelu_apprx_tanh, [PB[bk]], [b("gg")])
            if c < 3:
                load_wl(c + 1)
            for half in range(S // LT):
                t0 = half * LT
                pr_ = ti_ % 2
                ti_ += 1
                y, r, ii, t1, xcb = y2[pr_], r2[pr_], ii2[pr_], t12[pr_], xcb2[pr_]
                yB, rB, iB, tB, xcB = b(f"y{pr_}"), b(f"r{pr_}"), b(f"i{pr_}"), b(f"t1{pr_}"), b(f"xcb{pr_}")
                TS("dve", y, xraw[:, t0:t0 + LT], cw[:, c, 0:1], cb[:, c:c + 1], ALU.mult, ALU.add, [b("xraw")], [yB])
                for k in range(1, 4):
                    STT(y, xraw[:, t0 + k:t0 + k + LT], cw[:, c, k:k + 1], y, ALU.mult, ALU.add, [b("xraw"), yB], [yB])
                CP("pool", xcb, y, [yB], [xcB])
                for sub in range(LT // 512):
                    for which, dst, dB, bias in ((0, r, rB, br), (1, ii, iB, bi)):
                        bk = bi_ % 4
                        bi_ += 1
                        MM(bank(bk), wbd[:, c, which, :], xcb[:, sub * 512:(sub + 1) * 512], True, True, [xcB] + WBD, [PB[bk]])
                        ACT(dst[:, sub * 512:(sub + 1) * 512], bank(bk), AF.Sigmoid, [PB[bk]], [dB], bias=bias[:, c:c + 1], scale=1.0)
                ACT(r, r, AF.Exp, [rB], [rB], scale=clam[:, c:c + 1])
                TT("pool", t1, r, r, ALU.mult, [rB], [tB])
                ACT(t1, t1, AF.Sqrt, [tB], [tB], scale=-1.0, bias=1.0)
                TT("pool", ii, ii, y, ALU.mult, [iB, yB], [iB])
                TT("dve", t1, t1, ii, ALU.mult, [tB, iB], [tB])
                init = 0.0 if half == 0 else hprev[:, 0:1]
                S_.op("dve", lambda e, init=init, y=y, r=r, t1=t1: e.tensor_tensor_scan(out=y, data0=r, data1=t1, initial=init,
                                                                       op0=ALU.mult, op1=ALU.add),
                      [rB, tB, b("hprev")], [yB])
                CP("dve", hprev[:, 0:1], y[:, LT - 1:LT], [yB], [b("hprev")])
                TT("dve", oT[2][:, c, t0:t0 + LT], y, gg[:, t0:t0 + LT], ALU.mult, [yB, b("gg")], [OB[2]])
        S_.barrier()

    def swa_phase(l):
        win = din["w_in"][l]
        for j in range(2):
            wa = Alloc(W_BASE)
            wsw_all = [wa([128, 8, 384], BF16) for _ in range(2)]
            wsw = wsw_all[j]
            qkT = wa([128, 3, S], BF16)
            vaug = wa([128, NB, 66], BF16)
            sqv2 = [wa([128, 320], F32) for _ in range(2)]
            ssq2 = [wa([128, 8], F32) for _ in range(2)]
            tmpq2 = [wa([128, 320], F32) for _ in range(2)]
            qn = [wa([128, 384], BF16) for _ in range(2)]
            pex = [wa([128, 512], BF16) for _ in range(4)]
            oat = [wa([128, 256], BF16) for _ in range(2)]
            den = wa([128, 8], F32)
            wB = b(f"wsw{j}")
            rr = lambda a: a.rearrange("(kc p) n -> p kc n", p=128)
            if j == 0:
                for j2 in range(2):
                    LD("pool", wsw_all[j2][:, :, 0:256], rr(win[:, O_QA + j2 * 256:O_QA + (j2 + 1) * 256]), [b(f"wsw{j2}")])
                    LD("pool", wsw_all[j2][:, :, 256:320], rr(win[:, O_KA + j2 * 64:O_KA + (j2 + 1) * 64]), [b(f"wsw{j2}")])
                    LD("pool", wsw_all[j2][:, :, 320:384], rr(win[:, O_VA + j2 * 64:O_VA + (j2 + 1) * 64]), [b(f"wsw{j2}")])
            vB, qkB = b("vaug"), b("qkT")
            MS("pool", vaug[:, :, 64:65], 1.0, [vB])
            for tb in range(NB):
                bk = tb % 4
                par = tb % 2
                sqv, ssq, tmpq = sqv2[par], ssq2[par], tmpq2[par]
                sqB, ssB_, tqB = b(f"sqv{par}"), b(f"ssq{par}"), b(f"tmpq{par}")
                tbk = 7 if par == 0 else 6
                tbv = psum[:, tbk, :].bitcast(BF16)
                for kc in range(8):
                    MM(psum[:, bk, 0:384], hT[:, kc, tb * 128:(tb + 1) * 128], wsw[:, kc, :], kc == 0, kc == 7, [HB, wB], [PB[bk]])
                ACT(sqv, psum[:, bk, 0:320], AF.Square, [PB[bk]], [sqB])
                S_.op("dve", lambda e, sqv=sqv, ssq=ssq: e.tensor_reduce(out=ssq[:, 0:5], in_=sqv.rearrange("p (h d) -> p h d", d=64), axis=AX.X, op=ALU.add),
                      [sqB], [ssB_])
                ACT(ssq[:, 0:5], ssq[:, 0:5], AF.Ln, [ssB_], [ssB_], scale=1.0 / HD, bias=EPS)
                ACT(ssq[:, 0:5], ssq[:, 0:5], AF.Exp, [ssB_], [ssB_], scale=-0.5)
                TT("dve", tmpq.rearrange("p (h d) -> p h d", d=64), psum[:, bk, 0:320].rearrange("p (h d) -> p h d", d=64),
                   ssq[:, 0:5].unsqueeze(2).to_broadcast([128, 5, 64]), ALU.mult, [PB[bk], ssB_], [tqB])
                qnB = b(f"qn{par}")
                TT("dve", qn[par][:, 0:256].rearrange("p (h d) -> p h d", d=64), tmpq[:, 0:256].rearrange("p (h d) -> p h d", d=64),
                   qg8.unsqueeze(1).to_broadcast([128, 4, 64]), ALU.mult, [tqB, SMALL[0]], [qnB])
                TT("dve", qn[par][:, 256:384].rearrange("p (h d) -> p h d", d=64), tmpq[:, 256:320].unsqueeze(1).to_broadcast([128, 2, 64]),
                   kg.unsqueeze(1).to_broadcast([128, 2, 64]), ALU.mult, [tqB, SMALL[1]], [qnB])
                S_.op("act", lambda e, tb=tb, bk=bk: e.copy(out=vaug[:, tb, 0:64], in_=psum[:, bk, 320:384]), [PB[bk]], [vB])
                for s3 in range(3):
                    TR(tbv[:, s3 * 128:(s3 + 1) * 128], qn[par][:, s3 * 128:(s3 + 1) * 128], identb, [qnB], [PB[tbk]])
                CP("dve", qkT[:, :, tb * 128:(tb + 1) * 128], tbv[:, 0:384].rearrange("p (c t) -> p c t", t=128), [PB[tbk]], [qkB])
            pi_ = 0
            if dbg == "swa1":
                S_.barrier()
                return
            for tb in range(NB):
                kbs = [tb - 1, tb] if tb > 0 else [tb]
                pxs = []
                for kb in kbs:
                    bkp = 2 + 2 * (pi_ % 2)
                    px = pex[pi_ % 4]
                    pB = b(f"pex{pi_ % 4}")
                    pi_ += 1
                    for hh in range(2):
                        MM(psum[:, bkp + hh, 0:256].rearrange("p (a t) -> p a t", t=128), qkT[hh * 64:(hh + 1) * 64, 2, kb * 128:(kb + 1) * 128],
                           qkT[hh * 64:(hh + 1) * 64, 0:2, tb * 128:(tb + 1) * 128], True, True, [qkB], [PB[bkp + hh]])
                    for hh in range(2):
                        MM(psum[:, bkp + hh, 0:256], identb, (mD if kb == tb else mP)[:, 0:256], False, True, [cB], [PB[bkp + hh]], skip=True)
                    ACT(px.rearrange("p (a n) -> p a n", n=256), psum[:, bkp:bkp + 2, 0:256], AF.Exp, [PB[bkp], PB[bkp + 1]], [pB])
                    pxs.append((px, pB, kb))
                par = tb % 2
                if dbg == "swa2":
                    continue
                pvb = 6 if par == 0 else 0
                tbk = 7 if par == 0 else 1
                tbv = psum[:, tbk, :].bitcast(BF16)
                dn = den[:, 4 * par:4 * par + 4]
                pv = psum[:, pvb, 0:264].rearrange("p (s d) -> p s d", d=66)
                for slot in range(4):
                    for n_, (px, pB, kb) in enumerate(pxs):
                        MM(pv[:, slot, 0:65], px[:, slot * 128:(slot + 1) * 128], vaug[:, kb, 0:65], n_ == 0, n_ == len(pxs) - 1,
                           [pB, vB], [PB[pvb]])
                dB = b(f"den{par}")
                for hh in range(2):
                    for cc in range(2):
                        s_ = hh * 2 + cc
                        hd = 4 * j + 2 * cc + hh
                        TT("dve", dn[:, s_:s_ + 1], pv[:, s_, 64:65], esk[:, hd:hd + 1], ALU.add, [PB[pvb], SMALL[2]], [dB])
                S_.op("dve", lambda e, dn=dn: e.reciprocal(out=dn, in_=dn), [dB], [dB])
                oB = b(f"oat{par}")
                for hh in range(2):
                    TT("dve", oat[par].rearrange("p (cc hh d) -> p hh cc d", hh=2, d=64)[:, hh], pv[:, 2 * hh:2 * hh + 2, 0:64],
                       dn[:, 2 * hh:2 * hh + 2].unsqueeze(2).to_broadcast([128, 2, 64]), ALU.mult, [PB[pvb], dB], [oB])
                for cc in range(2):
                    TR(tbv[:, cc * 128:(cc + 1) * 128], oat[par][:, cc * 128:(cc + 1) * 128], identb, [oB], [PB[tbk]])
                S_.op("act", lambda e, tb=tb, j=j, tbv=tbv: e.copy(out=oT[0][:, 2 * j:2 * j + 2, tb * 128:(tb + 1) * 128],
                                                                   in_=tbv[:, 0:256].rearrange("p (c t) -> p c t", t=128)), [PB[tbk]], [OB[0]])
            S_.barrier()
            if dbg == "swa3":
                return

    def sb_phase(l):
        win = din["w_in"][l]
        wa = Alloc(W_BASE)
        wsb = wa([128, 8, 3, 128], BF16)
        qT = wa([128, S], BF16)
        kT = wa([128, S], BF16)
        vtok = wa([128, NB, 128], BF16)
        spf = [wa([128, 512], F32) for _ in range(3)]
        spb = [wa([128, 512], BF16) for _ in range(3)]
        Wt = [wa([128, 512], BF16) for _ in range(3)]
        Ssum = wa([128, 512], F32)
        Ssb = [wa([128, 512], BF16) for _ in range(2)]
        rr = lambda a: a.rearrange("(kc p) n -> p kc n", p=128)
        wB = b("wsb")

        def load_w(c):
            for i3, off in enumerate((O_QB, O_KB, O_VB)):
                LD("pool", wsb[:, :, i3, :], rr(win[:, off + c * 128:off + (c + 1) * 128]), [wB])

        load_w(0)
        zi = 0
        for c in range(4):
            qB, kB_, vB = b("qT"), b("kT"), b("vtok")
            for tg in range(NG):
                for which in range(2):
                    bk = zi % 4
                    zi += 1
                    for kc in range(8):
                        MM(bank(bk), wsb[:, kc, which, :], hT[:, kc, tg * 512:(tg + 1) * 512], kc == 0, kc == 7, [wB, HB], [PB[bk]])
                    if which == 0:
                        ACT(qT[:, tg * 512:(tg + 1) * 512], bank(bk), AF.Copy, [PB[bk]], [qB], scale=0.125)
                    else:
                        CP("dve", kT[:, tg * 512:(tg + 1) * 512], bank(bk), [PB[bk]], [kB_])
            for t4 in range(NB // 4):
                bk = zi % 4
                zi += 1
                for t_ in range(4):
                    tb = t4 * 4 + t_
                    for kc in range(8):
                        MM(psum[:, bk, t_ * 128:(t_ + 1) * 128], hT[:, kc, tb * 128:(tb + 1) * 128], wsb[:, kc, 2, :], kc == 0, kc == 7,
                           [wB, HB], [PB[bk]])
                CP("dve", vtok[:, t4 * 4:(t4 + 1) * 4, :], bank(bk).rearrange("p (t n) -> p t n", n=128), [PB[bk]], [vB])
            if c < 3:
                load_w(c + 1)
            for hh in range(2):
                p0, p1 = hh * 64, (hh + 1) * 64
                for qc in range(NG):
                    kbs = list(range(4 * qc + 3, -1, -1))
                    n = len(kbs)
                    pob = 4 + ((hh * NG + qc) % 2)
                    st = {}

                    def stage1(i):
                        kb = kbs[i]
                        bk = zi_base[0] % 4
                        zi_base[0] += 1
                        st[i] = bk
                        diag = kb >= 4 * qc
                        sB, bB = b(f"spf{i % 3}"), b(f"spb{i % 3}")
                        MM(bank(bk), kT[p0:p1, kb * 128:(kb + 1) * 128], qT[p0:p1, qc * 512:(qc + 1) * 512], True, True, [kB_, qB], [PB[bk]])
                        if diag:
                            MM(bank(bk), identb, sbm[:, kb - 4 * qc, :], False, True, [cB], [PB[bk]], skip=True)
                        ACT(spf[i % 3], bank(bk), AF.Exp, [PB[bk]], [sB])
                        ACT(spb[i % 3], spf[i % 3], AF.Ln, [sB], [bB], bias=1.0)

                    def stage2(i):
                        kb = kbs[i]
                        bk = st[i]
                        diag = kb >= 4 * qc
                        bB = b(f"spb{i % 3}")
                        wtB = b(f"Wt{i % 3}")
                        MM(bank(bk), Uneg, spb[i % 3], False, True, [bB], [PB[bk]], skip=True)
                        if i > 0:
                            MM(bank(bk), onesneg, Ssb[i % 2], False, True, [b(f"Ssb{i % 2}")], [PB[bk]], skip=True)
                        ACT(Wt[i % 3], bank(bk), AF.Exp, [PB[bk]], [wtB])
                        if i < n - 1:
                            if i == 0:
                                CP("dve", Ssum, spb[i % 3], [bB], [b("Ssum")])
                            else:
                                TT("dve", Ssum, Ssum, spb[i % 3], ALU.add, [b("Ssum"), bB], [b("Ssum")])
                            CP("dve", Ssb[(i + 1) % 2], Ssum, [b("Ssum")], [b(f"Ssb{(i + 1) % 2}")])

                    def stage3(i):
                        kb = kbs[i]
                        MM(psum[p0:p1, pob, :], vtok[:, kb, p0:p1], Wt[i % 3], i == 0, i == n - 1, [vB, b(f"Wt{i % 3}")], [PB[pob]])

                    zi_base = [zi]
                    for step in range(n + 2):
                        if step < n:
                            stage1(step)
                        if 0 <= step - 1 < n:
                            stage2(step - 1)
                        if 0 <= step - 2 < n:
                            stage3(step - 2)
                    zi = zi_base[0]
                    S_.op("act", lambda e, p0=p0, p1=p1, pob=pob, c=c, qc=qc: e.copy(
                        out=oT[1][p0:p1, c, qc * 512:(qc + 1) * 512], in_=psum[p0:p1, pob, :]), [PB[pob]], [OB[1]])
        S_.barrier()

    def merge_phase(l):
        win = din["w_in"][l]
        wps = [din["w_proj_a"][l], din["w_proj_b"][l], din["w_proj_c"][l]]
        wa = Alloc(W_BASE)
        gw = [wa([128, 8, 3, 128], BF16) for _ in range(2)]
        pw = [wa([128, 4, 3, 128], BF16) for _ in range(2)]
        ow = [wa([128, D], BF16) for _ in range(2)]
        sg = [wa([128, 512], F32) for _ in range(3)]
        tt = [wa([128, 512], F32) for _ in range(2)]
        mT = [wa([128, 512], BF16) for _ in range(2)]
        rr = lambda a: a.rearrange("(kc p) n -> p kc n", p=128)

        def load_gp(m):
            wB = b(f"mwg{m % 2}")
            for i in range(3):
                LD("pool", gw[m % 2][:, :, i, :], rr(win[:, O_GT + i * D + m * 128:O_GT + i * D + (m + 1) * 128]), [wB])
                LD("pool", pw[m % 2][:, :, i, :], rr(wps[i][:, m * 128:(m + 1) * 128]), [wB])

        def load_o(m):
            LD("pool", ow[m % 2], din["w_out"][l][m * 128:(m + 1) * 128, :], [b(f"mwo{m % 2}")])

        cnt = {"gi": 0, "oi": 0}

        def GP(m, tg, sp_):
            wB = b(f"mwg{m % 2}")
            tsl = slice(tg * 512, (tg + 1) * 512)
            for i in range(3):
                bk = cnt["gi"] % 4
                cnt["gi"] += 1
                for kc in range(8):
                    MM(bank(bk), gw[m % 2][:, kc, i, :], hT[:, kc, tsl], kc == 0, kc == 7, [wB, HB], [PB[bk]])
                ACT(sg[i], bank(bk), AF.Sigmoid, [PB[bk], SMALL[11]], [b(f"sg{i}")], bias=bg[:, i * 8 + m:i * 8 + m + 1], scale=1.0)
            mB = b(f"mT{sp_ % 2}")
            for i in range(3):
                bk = cnt["gi"] % 4
                cnt["gi"] += 1
                for kc in range(4):
                    MM(bank(bk), pw[m % 2][:, kc, i, :], oT[i][:, kc, tsl], kc == 0, kc == 3, [wB, OB[i]], [PB[bk]])
                if i == 0:
                    TT("dve", tt[0], sg[0], bank(bk), ALU.mult, [b("sg0"), PB[bk]], [b("tt0")])
                elif i == 1:
                    TT("dve", tt[1], sg[1], bank(bk), ALU.mult, [b("sg1"), PB[bk]], [b("tt1")])
                    TT("pool", tt[0], tt[0], tt[1], ALU.add, [b("tt0"), b("tt1")], [b("tt0")])
                else:
                    TT("dve", tt[1], sg[2], bank(bk), ALU.mult, [b("sg2"), PB[bk]], [b("tt1")])
                    TT("pool", mT[sp_ % 2], tt[0], tt[1], ALU.add, [b("tt0"), b("tt1")], [mB])

        def OUT(m, tg, sp_):
            mB = b(f"mT{sp_ % 2}")
            for t_ in range(4):
                tb = tg * 4 + t_
                for cg in range(2):
                    bk = 4 + (cnt["oi"] % 4)
                    cnt["oi"] += 1
                    MM(bank(bk), mT[sp_ % 2][:, t_ * 128:(t_ + 1) * 128], ow[m % 2][:, cg * 512:(cg + 1) * 512], True, True,
                       [mB, b(f"mwo{m % 2}")], [PB[bk]])
                    xs = xres[:, tb, cg * 512:(cg + 1) * 512]
                    TT("dve", xs, xs, bank(bk), ALU.add, [XB[tb][cg], PB[bk]], [XB[tb][cg]])

        load_gp(0)
        load_o(0)
        prev = None
        for m in range(8):
            if m < 7:
                load_gp(m + 1)
            for tg in range(NG):
                GP(m, tg, m * NG + tg)
                if prev is not None:
                    OUT(*prev)
                if tg == 0 and m < 7:
                    load_o(m + 1)
                prev = (m, tg, m * NG + tg)
        OUT(*prev)
        S_.barrier()

    class FFN:
        def __init__(self, units, base):
            wa = Alloc(base)
            self.wg = [wa([128, 8, 512], BF16) for _ in range(2)]
            self.wu = [wa([128, 8, 512], BF16) for _ in range(2)]
            self.wd = [wa([128, 4, D], BF16) for _ in range(2)]
            self.sg = [wa([128, 512], F32) for _ in range(2)]
            self.act = [wa([128, 4, 512], BF16) for _ in range(2)]
            self.end = wa.off
            NFG = DFF // 512
            self.items = [(u, fg) for u in units for fg in range(NFG)]

        def load(self, n):
            (wg_d, wu_d, wd_d, _e), fg = self.items[n]
            rr = lambda a: a.rearrange("(kc p) n -> p kc n", p=128)
            wB = b(f"fw{n % 2}")
            LD("pool", self.wg[n % 2], rr(wg_d[:, fg * 512:(fg + 1) * 512]), [wB])
            LD("pool", self.wu[n % 2], rr(wu_d[:, fg * 512:(fg + 1) * 512]), [wB])
            LD("pool", self.wd[n % 2], rr(wd_d[fg * 512:(fg + 1) * 512, :]), [wB])

        def run(self):
            gi = oi = ai = 0
            for n, ((wg_d, wu_d, wd_d, e_idx), fg) in enumerate(self.items):
                if n + 1 < len(self.items):
                    self.load(n + 1)
                wB = b(f"fw{n % 2}")
                wg, wu, wd = self.wg[n % 2], self.wu[n % 2], self.wd[n % 2]
                for tg in range(NG):
                    tsl = slice(tg * 512, (tg + 1) * 512)
                    aB = b(f"act{ai % 2}")
                    a_ = self.act[ai % 2]
                    ai += 1
                    for fc in range(4):
                        bg_ = gi % 4
                        gi += 1
                        bu_ = gi % 4
                        gi += 1
                        sB = b(f"fsg{fc % 2}")
                        for kc in range(8):
                            MM(bank(bg_), wg[:, kc, fc * 128:(fc + 1) * 128], hT[:, kc, tsl], kc == 0, kc == 7, [wB, HB], [PB[bg_]])
                        for kc in range(8):
                            MM(bank(bu_), wu[:, kc, fc * 128:(fc + 1) * 128], hT[:, kc, tsl], kc == 0, kc == 7, [wB, HB], [PB[bu_]])
                        ACT(self.sg[fc % 2], bank(bg_), AF.Silu, [PB[bg_]], [sB])
                        TT("dve", a_[:, fc, :], self.sg[fc % 2], bank(bu_), ALU.mult, [sB, PB[bu_]], [aB])
                    for t_ in range(4):
                        tb = tg * 4 + t_
                        for cg in range(2):
                            bk = 4 + (oi % 4)
                            oi += 1
                            for fc in range(4):
                                MM(bank(bk), a_[:, fc, t_ * 128:(t_ + 1) * 128], wd[:, fc, cg * 512:(cg + 1) * 512], fc == 0, fc == 3,
                                   [aB, wB], [PB[bk]])
                            xs = xres[:, tb, cg * 512:(cg + 1) * 512]
                            if e_idx is None:
                                TT("dve", xs, xs, bank(bk), ALU.add, [XB[tb][cg], PB[bk]], [XB[tb][cg]])
                            else:
                                STT(xs, bank(bk), comb[:, tb, e_idx:e_idx + 1], xs, ALU.mult, ALU.add,
                                    [XB[tb][cg], PB[bk], b("comb")], [XB[tb][cg]])
            S_.barrier()

    oB = b("out")

    def dump(br_):
        stg = view(W_BASE, [128, 4 * S], F32)
        CP("dve", stg, oT[br_].rearrange("p c s -> p (c s)"), [OB[br_]], [b("stg")])
        S_.barrier()
        LD("sp", out[0].rearrange("(p a) d -> p (a d)", p=128)[:, 0:4 * S], stg, [b("dumped")])
        S_.barrier()

    for seq in range(NSEQ):
        xv = din["x"][seq].rearrange("(tb p) d -> p tb d", p=128)
        for q4 in range(0, NB, 4):
            LD("sp", xres[:, q4:q4 + 4, :], xv[:, q4:q4 + 4, :], [XB[tb][cg] for tb in range(q4, q4 + 4) for cg in range(2)])
        for l in range(DEPTH):
            load_layer_consts(l)
            norm_phase(din["attn_norm"][l], W_BASE)
            if dbg == "hT":
                break
            lru_phase(l)
            if dbg == "lru":
                dump(2)
                break
            swa_phase(l)
            if dbg in ("swa", "swa1", "swa2", "swa3"):
                dump(0)
                break
            sb_phase(l)
            if dbg == "sb":
                dump(1)
                break
            merge_phase(l)
            if dbg == "mixer":
                break
            j = l // 2
            if l % 2 == 0:
                units = [(din["w_ffn_gate"][j], din["w_ffn_up"][j], din["w_ffn_down"][j], None)]
            else:
                units = [(din["w_exp_gate"][j, e], din["w_exp_up"][j, e], din["w_exp_down"][j, e], e) for e in range(NE)]
            ffn = FFN(units, O_BASE)
            ffn.load(0)
            norm_phase(din["ffn_norm"][l], ffn.end, router_w=(din["w_router"][j] if l % 2 else None))
            ffn.run()
        ov = out[seq].rearrange("(tb p) d -> p tb d", p=128)
        for q4 in range(0, NB, 4):
            if dbg in ("lru", "swa", "sb", "swa1", "swa2", "swa3"):
                break
            S_.dma("sp", ov[:, q4:q4 + 4, :], xres[:, q4:q4 + 4, :], [XB[tb][cg] for tb in range(q4, q4 + 4) for cg in range(2)], [oB])
        S_.barrier()
    S_.barrier()
    S_.emit()
    return nc


_NC_CACHE = {}


def kernel(**inputs):
    x = np.ascontiguousarray(inputs["x"], dtype=np.float32)
    BATCH = x.shape[0]
    per = BATCH // NCORES
    if "nc" not in _NC_CACHE:
        _NC_CACHE["nc"] = build(S=x.shape[1], NSEQ=per, DEPTH=inputs["w_in"].shape[0])
    nc = _NC_CACHE["nc"]
    shared = {k: np.ascontiguousarray(v, dtype=np.float32) for k, v in inputs.items() if k != "x"}
    in_maps = []
    for i in range(NCORES):
        m = dict(shared)
        m["x"] = np.ascontiguousarray(x[i * per:(i + 1) * per])
        in_maps.append(m)
    res = run_bass_kernel_spmd(nc, in_maps, core_ids=list(range(NCORES)))
    return np.concatenate([np.asarray(r["out"]) for r in res.results], axis=0).astype(np.float32)
```

```python
import numpy as np
import concourse.bass as bass
import concourse.mybir as mybir
from concourse.bass_utils import run_bass_kernel_spmd

F32 = mybir.dt.float32
BF16 = mybir.dt.bfloat16
AF = mybir.ActivationFunctionType
ALU = mybir.AluOpType
AX = mybir.AxisListType

D = 1024
HD = 64
INC = 6400
DFF = 3584
NE = 8
EPS = 1e-6
O_QA, O_KA, O_VA, O_QB, O_KB, O_VB, O_XC, O_GC, O_GT = 0, 512, 640, 768, 1280, 1792, 2304, 2816, 3328
NCORES = 8


class Buf:
    __slots__ = ("name", "w", "rs")

    def __init__(self, name):
        self.name = name
        self.w = None
        self.rs = {}


class Sched:
    SEM_LIMIT = 30000
    NDMA = 6

    def __init__(self, nc):
        self.nc = nc
        self.names = ["pe", "act", "dve", "pool", "sp"]
        self.lists = {k: [] for k in self.names}
        self.seen = {k: {} for k in self.names}
        self.cur = {}
        self.sems = {}
        self.last = {}
        self.dma_slots = {}
        self.dma_rr = {k: 0 for k in self.names}
        self.ctxs = []

    def _newsem(self, name):
        cm = self.nc.semaphore(name)
        self.sems[name] = cm.__enter__()
        self.ctxs.append(cm)
        return name

    def _eng_event(self, eng):
        c = self.cur.get(eng)
        if c is None or c[1] >= self.SEM_LIMIT:
            ep = 0 if c is None else c[2] + 1
            c = [self._newsem(f"s_{eng}_{ep}"), 0, ep]
            self.cur[eng] = c
        c[1] += 1
        self.last[c[0]] = c[1]
        return (c[0], c[1])

    def _need(self, eng, evs):
        out = {}
        for ev in evs:
            if ev is None:
                continue
            k, v = ev
            if self.seen[eng].get(k, 0) >= v:
                continue
            if out.get(k, 0) < v:
                out[k] = v
        for k, v in out.items():
            self.seen[eng][k] = v
        return list(out.items())

    def _deps(self, eng, reads, writes):
        c = self.cur.get(eng)
        own = c[0] if c else None
        evs = []
        for b in reads:
            evs.append(b.w)
        for b in writes:
            evs.append(b.w)
            evs.extend(b.rs.items())
        if eng == "pe":
            evs = [e for e in evs if e is not None and e[0] != own]
        return self._need(eng, evs)

    def _commit(self, ev, reads, writes):
        for b in reads:
            if b.rs.get(ev[0], 0) < ev[1]:
                b.rs[ev[0]] = ev[1]
        for b in writes:
            b.w = ev
            b.rs = {}

    def op(self, eng, fn, reads=(), writes=()):
        waits = self._deps(eng, reads, writes)
        ev = self._eng_event(eng)
        self.lists[eng].append((waits, fn, ev, 1))
        self._commit(ev, reads, writes)
        return ev

    def dma(self, q, out, in_, reads=(), writes=(), **kw):
        slots = self.dma_slots.setdefault(q, [])
        if len(slots) < self.NDMA:
            slots.append([self._newsem(f"d_{q}_{len(slots)}"), 0])
        slot = slots[self.dma_rr[q] % self.NDMA]
        self.dma_rr[q] += 1
        waits = self._deps(q, reads, writes)
        if slot[1] > 0:
            waits += self._need(q, [(slot[0], slot[1])])
        slot[1] += 16
        ev = (slot[0], slot[1])
        self.last[slot[0]] = slot[1]
        fn = lambda e: e.dma_start(out=out, in_=in_, **kw)
        self.lists[q].append((waits, fn, ev, 16))
        self._commit(ev, reads, writes)
        return ev

    def barrier(self):
        evs = list(self.last.items())
        for eng in self.names:
            waits = self._need(eng, evs)
            if waits:
                self.lists[eng].append((waits, None, None, 0))

    def emit(self):
        nc = self.nc
        engs = {"pe": "tensor", "act": "scalar", "dve": "vector", "pool": "gpsimd", "sp": "sync"}
        with nc.Block() as block:
            def mk(name):
                def body(e):
                    for waits, fn, ev, inc in self.lists[name]:
                        for k, v in waits:
                            e.wait_ge(self.sems[k], v)
                        if fn is not None:
                            fn(e).then_inc(self.sems[ev[0]], inc)
                return body
            for name in self.names:
                getattr(block, engs[name])(mk(name))
        for cm in reversed(self.ctxs):
            cm.__exit__(None, None, None)


def build(S=2048, NSEQ=2, DEPTH=2, dbg=None):
    NB = S // 128
    NG = S // 512
    LT = 512
    nc = bass.Bass("TRN2", target_bir_lowering=False)
    din = {}

    def dram_in(name, shape):
        din[name] = nc.dram_tensor(name, list(shape), F32, kind="ExternalInput").ap()

    NDENSE = (DEPTH + 1) // 2
    NMOE = max(DEPTH // 2, 1)
    dram_in("x", [NSEQ, S, D])
    dram_in("attn_norm", [DEPTH, D]); dram_in("w_in", [DEPTH, D, INC]); dram_in("b_gate", [DEPTH, 3 * D])
    dram_in("q_norm", [DEPTH, HD]); dram_in("k_norm", [DEPTH, HD]); dram_in("sinks", [DEPTH, 8])
    dram_in("conv_w", [DEPTH, 4, 512]); dram_in("conv_b", [DEPTH, 512])
    dram_in("lru_w_r", [DEPTH, 8, 64, 64]); dram_in("lru_b_r", [DEPTH, 512])
    dram_in("lru_w_i", [DEPTH, 8, 64, 64]); dram_in("lru_b_i", [DEPTH, 512]); dram_in("lru_lambda", [DEPTH, 512])
    dram_in("w_proj_a", [DEPTH, 512, D]); dram_in("w_proj_b", [DEPTH, 512, D]); dram_in("w_proj_c", [DEPTH, 512, D])
    dram_in("w_out", [DEPTH, D, D]); dram_in("ffn_norm", [DEPTH, D])
    dram_in("w_ffn_gate", [NDENSE, D, DFF]); dram_in("w_ffn_up", [NDENSE, D, DFF]); dram_in("w_ffn_down", [NDENSE, DFF, D])
    dram_in("w_router", [NMOE, D, NE]); dram_in("w_exp_gate", [NMOE, NE, D, DFF])
    dram_in("w_exp_up", [NMOE, NE, D, DFF]); dram_in("w_exp_down", [NMOE, NE, DFF, D])
    out = nc.dram_tensor("out", [NSEQ, S, D], F32, kind="ExternalOutput").ap()

    S_ = Sched(nc)
    ARENA = 208000
    arena = nc.alloc_sbuf_tensor("arena", [128, ARENA // 4], F32).ap()
    psum = nc.alloc_psum_tensor("psum", [128, 8, 512], F32).ap()

    def bank(i):
        return psum[:, i, :]

    def view(off, shape, dt):
        n = int(np.prod(shape[1:]))
        sz = 4 if dt == F32 else 2
        assert off % 4 == 0 and (n * sz) % 4 == 0, (off, shape)
        assert off + n * sz <= ARENA, ("arena overflow", off, shape)
        w = arena[:, off // 4:(off + n * sz) // 4]
        v = w if dt == F32 else w.bitcast(dt)
        if len(shape) == 3:
            v = v.rearrange("p (a b) -> p a b", b=shape[2])
        elif len(shape) == 4:
            v = v.rearrange("p (a b c) -> p a b c", b=shape[2], c=shape[3])
        return v

    class Alloc:
        def __init__(self, base):
            self.off = base

        def __call__(self, shape, dt):
            n = int(np.prod(shape[1:])) * (4 if dt == F32 else 2)
            n = (n + 31) // 32 * 32
            v = view(self.off, shape, dt)
            self.off += n
            return v

    pa = Alloc(0)
    identb = pa([128, 128], BF16)
    identf = pa([128, 128], F32)
    onesb = pa([128, 128], BF16)
    Uneg = pa([128, 128], BF16)
    onesneg = pa([128, 128], BF16)
    mD = pa([128, 512], BF16)
    mP = pa([128, 512], BF16)
    sbm = pa([128, 4, 512], BF16)
    gt = pa([128, D], F32)
    LC = []
    for _l in range(DEPTH):
        LC.append(dict(qg8=pa([128, HD], F32), kg=pa([128, HD], F32), esk=pa([128, 8], F32), cw=pa([128, 4, 4], F32),
                       cb=pa([128, 4], F32), br=pa([128, 4], F32), bi=pa([128, 4], F32), clam=pa([128, 4], F32),
                       bg=pa([128, 24], F32), wbd=pa([128, 4, 2, 128], BF16)))
    qg8 = kg = esk = cw = cb = br = bi = clam = bg = wbd = None

    def set_layer(l):
        nonlocal qg8, kg, esk, cw, cb, br, bi, clam, bg, wbd
        d_ = LC[l]
        qg8, kg, esk, cw, cb, br, bi, clam, bg, wbd = (d_["qg8"], d_["kg"], d_["esk"], d_["cw"], d_["cb"], d_["br"], d_["bi"],
                                                       d_["clam"], d_["bg"], d_["wbd"])
    comb = pa([128, NB, NE], F32)
    hprev = pa([128, 2], F32)
    xres = pa([128, NB, D], F32)
    hT = pa([128, 8, S], BF16)
    O_BASE = pa.off
    oT = [pa([128, 4, S], BF16) for _ in range(3)]
    W_BASE = pa.off

    B = {}

    def b(name):
        if name not in B:
            B[name] = Buf(name)
        return B[name]

    PB = [b(f"bank{i}") for i in range(8)]

    def MM(out, lhsT, rhs, start, stop, reads, writes, skip=False):
        S_.op("pe", lambda e: e.matmul(out=out, lhsT=lhsT, rhs=rhs, start=start, stop=stop, skip_group_check=skip), reads, writes)

    def TR(out, in_, ident, reads, writes):
        S_.op("pe", lambda e: e.transpose(out=out, in_=in_, identity=ident), reads, writes)

    def ACT(out, in_, func, reads, writes, **kw):
        S_.op("act", lambda e: e.activation(out=out, in_=in_, func=func, **kw), reads, writes)

    def TT(eng, out, in0, in1, op, reads, writes):
        S_.op(eng, lambda e: e.tensor_tensor(out=out, in0=in0, in1=in1, op=op), reads, writes)

    def TS(eng, out, in0, s1, s2, op0, op1, reads, writes):
        S_.op(eng, lambda e: e.tensor_scalar(out=out, in0=in0, scalar1=s1, scalar2=s2, op0=op0, op1=op1), reads, writes)

    def STT(out, in0, scalar, in1, op0, op1, reads, writes):
        S_.op("dve", lambda e: e.scalar_tensor_tensor(out=out, in0=in0, scalar=scalar, in1=in1, op0=op0, op1=op1), reads, writes)

    def CP(eng, out, in_, reads, writes):
        S_.op(eng, lambda e: e.tensor_copy(out=out, in_=in_), reads, writes)

    def MS(eng, ap, val, writes):
        S_.op(eng, lambda e: e.memset(ap, val), (), writes)

    def ASEL(out, in_, pattern, cmp, fill, base, cm, reads, writes):
        S_.op("pool", lambda e: e.affine_select(out=out, in_=in_, pattern=pattern, compare_op=cmp, fill=fill,
                                                base=base, channel_multiplier=cm), reads, writes)

    def LD(q, out_, in_, writes, **kw):
        S_.dma(q, out_, in_, (), writes, **kw)

    bank7b = psum[:, 7, :].bitcast(BF16)

    tmpc = view(W_BASE, [128, 512], F32)
    cB = b("consts")
    MS("pool", identf, 0.0, [cB])
    ASEL(identf, identf, [[-1, 128]], ALU.not_equal, 1.0, 0, 1, [cB], [cB])
    CP("dve", identb, identf, [cB], [cB])
    MS("pool", onesb, 1.0, [cB])
    MS("pool", onesneg, -1.0, [cB])
    MS("pool", tmpc[:, 0:128], -1.0, [cB])
    ASEL(tmpc[:, 0:128], tmpc[:, 0:128], [[-1, 128]], ALU.is_ge, 0.0, 0, 1, [cB], [cB])
    CP("dve", Uneg, tmpc[:, 0:128], [cB], [cB])
    MS("pool", tmpc, 1.0, [cB])
    ASEL(tmpc.rearrange("p (a t) -> p a t", t=128), tmpc.rearrange("p (a t) -> p a t", t=128), [[0, 4], [1, 128]],
         ALU.is_ge, 0.0, 0, -1, [cB], [cB])
    TS("dve", tmpc, tmpc, 30000.0, -30000.0, ALU.mult, ALU.add, [cB], [cB])
    CP("dve", mD, tmpc, [cB], [cB])
    MS("pool", tmpc, 1.0, [cB])
    ASEL(tmpc.rearrange("p (a t) -> p a t", t=128), tmpc.rearrange("p (a t) -> p a t", t=128), [[0, 4], [-1, 128]],
         ALU.is_gt, 0.0, 0, 1, [cB], [cB])
    TS("dve", tmpc, tmpc, 30000.0, -30000.0, ALU.mult, ALU.add, [cB], [cB])
    CP("dve", mP, tmpc, [cB], [cB])
    for k in range(4):
        MS("pool", tmpc, 1.0, [cB])
        ASEL(tmpc, tmpc, [[1, 512]], ALU.is_gt, 0.0, -128 * k, -1, [cB], [cB])
        TS("dve", tmpc, tmpc, 30000.0, -30000.0, ALU.mult, ALU.add, [cB], [cB])
        CP("dve", sbm[:, k, :], tmpc, [cB], [cB])
    S_.barrier()

    XB = [[b(f"x{tb}_{cg}") for cg in range(2)] for tb in range(NB)]
    HB = b("hT")
    OB = [b(f"oT{i}") for i in range(3)]

    def xbufs(tb):
        return XB[tb]

    SMALL = [b(n) for n in ("s_qg8", "s_kg", "s_esk", "s_cw0", "s_cw1", "s_cw2", "s_cw3", "s_cb", "s_br", "s_bi", "s_clam", "s_bg")]
    WBD = [b(f"s_wbd{i}") for i in range(16)]

    def load_layer_consts(l):
        NCD = dict(allow_slow_non_contiguous=True)
        sq_, sk_, se_, c0, c1, c2, c3, scb, sbr, sbi, scl, sbg = SMALL
        LD("sp", qg8, din["q_norm"][l].partition_broadcast(128), [sq_])
        LD("sp", kg, din["k_norm"][l].partition_broadcast(128), [sk_])
        LD("sp", esk, din["sinks"][l].partition_broadcast(128), [se_])
        for k, cB_ in enumerate((c0, c1, c2, c3)):
            LD("sp", cw[:, :, k], din["conv_w"][l, k].rearrange("(c p) -> p c", p=128), [cB_], **NCD)
        LD("sp", cb, din["conv_b"][l].rearrange("(c p) -> p c", p=128), [scb], **NCD)
        LD("sp", br, din["lru_b_r"][l].rearrange("(c p) -> p c", p=128), [sbr], **NCD)
        LD("sp", bi, din["lru_b_i"][l].rearrange("(c p) -> p c", p=128), [sbi], **NCD)
        LD("sp", clam, din["lru_lambda"][l].rearrange("(c p) -> p c", p=128), [scl], **NCD)
        LD("sp", bg, din["b_gate"][l].rearrange("(c p) -> p c", p=128), [sbg], **NCD)
        MS("pool", wbd, 0.0, WBD)
        n_ = 0
        for c in range(4):
            for hh in range(2):
                LD("pool", wbd[hh * 64:(hh + 1) * 64, c, 0, hh * 64:(hh + 1) * 64], din["lru_w_r"][l, 2 * c + hh], [WBD[n_]])
                LD("pool", wbd[hh * 64:(hh + 1) * 64, c, 1, hh * 64:(hh + 1) * 64], din["lru_w_i"][l, 2 * c + hh], [WBD[n_ + 1]])
                n_ += 2
        TS("dve", qg8, qg8, 0.125, None, ALU.mult, ALU.bypass, [sq_], [sq_])
        ACT(esk, esk, AF.Exp, [se_], [se_])
        ACT(clam, clam, AF.Exp, [scl], [scl], scale=-1.0)
        ACT(clam, clam, AF.Ln, [scl], [scl], bias=1.0)
        TS("dve", clam, clam, -8.0, None, ALU.mult, ALU.bypass, [scl], [scl])
        S_.barrier()

    def norm_phase(gain_dram, base, router_w=None):
        wa = Alloc(base)
        junk = wa([128, D], BF16)
        hb = [wa([128, D], BF16) for _ in range(2)]
        ss = wa([128, 4], F32)
        gB = b("gt")
        LD("sp", gt, gain_dram.partition_broadcast(128), [gB])
        if router_w is not None:
            hf2 = [wa([128, D], F32) for _ in range(2)]
            hTf2 = [wa([128, 8, 128], F32) for _ in range(2)]
            wr = wa([128, 8, NE], F32)
            lg2 = [wa([128, NE], F32) for _ in range(2)]
            t82 = [[wa([128, NE], F32) for _ in range(3)] for _ in range(2)]
            m122 = [wa([128, 4], F32) for _ in range(2)]
            LD("sp", wr, router_w.rearrange("(kc p) e -> p kc e", p=128), [b("wr")])
        for tb in range(NB):
            par = tb % 2
            xr = xres[:, tb, :]
            ssB, hbB = b(f"ss{par}"), b(f"hb{par}")
            ACT(junk, xr, AF.Square, xbufs(tb), [b("junk"), ssB], accum_out=ss[:, par:par + 1])
            ACT(ss[:, par:par + 1], ss[:, par:par + 1], AF.Ln, [ssB], [ssB], scale=1.0 / D, bias=EPS)
            ACT(ss[:, par:par + 1], ss[:, par:par + 1], AF.Exp, [ssB], [ssB], scale=-0.5)
            STT(hb[par], xr, ss[:, par:par + 1], gt, ALU.mult, ALU.mult, xbufs(tb) + [ssB, gB], [hbB])
            tbk = 7 if par == 0 else 3
            tbv = psum[:, tbk, :].bitcast(BF16)
            for c in range(8):
                TR(tbv[:, c * 128:(c + 1) * 128], hb[par][:, c * 128:(c + 1) * 128], identb, [hbB], [PB[tbk]])
            if tb % 2:
                CP("dve", hT[:, :, tb * 128:(tb + 1) * 128], tbv.rearrange("p (c t) -> p c t", t=128), [PB[tbk]], [HB])
            else:
                S_.op("act", lambda e, tb=tb, tbv=tbv: e.copy(out=hT[:, :, tb * 128:(tb + 1) * 128],
                                                              in_=tbv.rearrange("p (c t) -> p c t", t=128)), [PB[tbk]], [HB])
            if router_w is not None:
                hf, hTf, lg, t8, m12 = hf2[par], hTf2[par], lg2[par], t82[par], m122[par]
                tb0 = 5 if par == 0 else 1
                lbk = 4 if par == 0 else 0
                hfB, hTB, lB, mB_ = b(f"hf{par}"), b(f"hTf{par}"), b(f"lg{par}"), b(f"m12{par}")
                t0B, t1B, t2B = b(f"t80{par}"), b(f"t81{par}"), b(f"t82{par}")
                STT(hf, xr, ss[:, par:par + 1], gt, ALU.mult, ALU.mult, xbufs(tb) + [ssB, gB], [hfB])
                for c in range(8):
                    TR(psum[:, tb0 + c // 4, (c % 4) * 128:(c % 4 + 1) * 128], hf[:, c * 128:(c + 1) * 128], identf,
                       [hfB], [PB[tb0 + c // 4]])
                CP("dve", hTf, psum[:, tb0:tb0 + 2, :].rearrange("p a (c t) -> p (a c) t", t=128), [PB[tb0], PB[tb0 + 1]], [hTB])
                for c in range(8):
                    MM(psum[:, lbk, 0:NE], hTf[:, c, :], wr[:, c, :], c == 0, c == 7, [hTB, b("wr")], [PB[lbk]])
                CP("dve", lg, psum[:, lbk, 0:NE], [PB[lbk]], [lB])
                S_.op("dve", lambda e, m12=m12, lg=lg: e.reduce_max(out=m12[:, 0:1], in_=lg, axis=AX.X), [lB], [mB_])
                TS("dve", t8[0], lg, m12[:, 0:1], None, ALU.is_equal, ALU.bypass, [lB, mB_], [t0B])
                STT(t8[1], t8[0], -1e30, lg, ALU.mult, ALU.add, [t0B, lB], [t1B])
                S_.op("dve", lambda e, m12=m12, t8=t8: e.reduce_max(out=m12[:, 1:2], in_=t8[1], axis=AX.X), [t1B], [mB_])
                TS("dve", t8[0], lg, m12[:, 1:2], None, ALU.is_ge, ALU.bypass, [lB, mB_], [t0B])
                TS("dve", m12[:, 2:3], m12[:, 0:1], -1.0, None, ALU.mult, ALU.bypass, [mB_], [mB_])
                ACT(t8[1], lg, AF.Exp, [lB, mB_], [t1B], bias=m12[:, 2:3], scale=1.0)
                TT("dve", t8[2], t8[1], t8[0], ALU.mult, [t1B, t0B], [t2B])
                S_.op("dve", lambda e, m12=m12, t8=t8: e.reduce_sum(out=m12[:, 3:4], in_=t8[2], axis=AX.X), [t2B], [mB_])
                S_.op("dve", lambda e, m12=m12: e.reciprocal(out=m12[:, 3:4], in_=m12[:, 3:4]), [mB_], [mB_])
                TS("dve", comb[:, tb, :], t8[2], m12[:, 3:4], None, ALU.mult, ALU.bypass, [t2B, mB_], [b("comb")])
        S_.barrier()

    def lru_phase(l):
        wa = Alloc(W_BASE)
        xraw = wa([128, S + 4], F32)
        gg = wa([128, S], BF16)
        y2 = [wa([128, LT], F32) for _ in range(2)]
        r2 = [wa([128, LT], F32) for _ in range(2)]
        ii2 = [wa([128, LT], F32) for _ in range(2)]
        t12 = [wa([128, LT], F32) for _ in range(2)]
        xcb2 = [wa([128, LT], BF16) for _ in range(2)]
        ti_ = 0
        wl = [wa([128, 8, 2, 128], BF16) for _ in range(2)]
        win = din["w_in"][l]
        MS("pool", xraw[:, 0:3], 0.0, [b("xraw")])
        bi_ = 0
        def load_wl(c):
            LD("pool", wl[c % 2][:, :, 0, :], win[:, O_XC + c * 128:O_XC + (c + 1) * 128].rearrange("(kc p) n -> p kc n", p=128), [b(f"wl{c % 2}")])
            LD("pool", wl[c % 2][:, :, 1, :], win[:, O_GC + c * 128:O_GC + (c + 1) * 128].rearrange("(kc p) n -> p kc n", p=128), [b(f"wl{c % 2}")])

        load_wl(0)
        for c in range(4):
            wB = b(f"wl{c % 2}")
            for tg in range(NG):
                for which in range(2):
                    bk = bi_ % 4
                    bi_ += 1
                    for kc in range(8):
                        MM(bank(bk), wl[c % 2][:, kc, which, :], hT[:, kc, tg * 512:(tg + 1) * 512], kc == 0, kc == 7, [wB, HB], [PB[bk]])
                    if which == 0:
                        S_.op("act", lambda e, bk=bk, tg=tg: e.copy(out=xraw[:, 3 + tg * 512:3 + (tg + 1) * 512], in_=bank(bk)), [PB[bk]], [b("xraw")])
                    else:
                        ACT(gg[:, tg * 512:(tg + 1) * 512], bank(bk), AF.Gelu_apprx_tanh, [PB[bk]], [b("gg")])
            if c < 3:
                load_wl(c + 1)
            for half in range(S // LT):
                t0 = half * LT
                pr_ = ti_ % 2
                ti_ += 1
                y, r, ii, t1, xcb = y2[pr_], r2[pr_], ii2[pr_], t12[pr_], xcb2[pr_]
                yB, rB, iB, tB, xcB = b(f"y{pr_}"), b(f"r{pr_}"), b(f"i{pr_}"), b(f"t1{pr_}"), b(f"xcb{pr_}")
                TS("dve", y, xraw[:, t0:t0 + LT], cw[:, c, 0:1], cb[:, c:c + 1], ALU.mult, ALU.add, [b("xraw")], [yB])
                for k in range(1, 4):
                    STT(y, xraw[:, t0 + k:t0 + k + LT], cw[:, c, k:k + 1], y, ALU.mult, ALU.add, [b("xraw"), yB], [yB])
                CP("pool", xcb, y, [yB], [xcB])
                for sub in range(LT // 512):
                    for which, dst, dB, bias in ((0, r, rB, br), (1, ii, iB, bi)):
                        bk = bi_ % 4
                        bi_ += 1
                        MM(bank(bk), wbd[:, c, which, :], xcb[:, sub * 512:(sub + 1) * 512], True, True, [xcB] + WBD, [PB[bk]])
                        ACT(dst[:, sub * 512:(sub + 1) * 512], bank(bk), AF.Sigmoid, [PB[bk]], [dB], bias=bias[:, c:c + 1], scale=1.0)
                ACT(r, r, AF.Exp, [rB], [rB], scale=clam[:, c:c + 1])
                TT("pool", t1, r, r, ALU.mult, [rB], [tB])
                ACT(t1, t1, AF.Sqrt, [tB], [tB], scale=-1.0, bias=1.0)
                TT("pool", ii, ii, y, ALU.mult, [iB, yB], [iB])
                TT("dve", t1, t1, ii, ALU.mult, [tB, iB], [tB])
                init = 0.0 if half == 0 else hprev[:, 0:1]
                S_.op("dve", lambda e, init=init, y=y, r=r, t1=t1: e.tensor_tensor_scan(out=y, data0=r, data1=t1, initial=init,
                                                                       op0=ALU.mult, op1=ALU.add),
                      [rB, tB, b("hprev")], [yB])
                CP("dve", hprev[:, 0:1], y[:, LT - 1:LT], [yB], [b("hprev")])
                TT("dve", oT[2][:, c, t0:t0 + LT], y, gg[:, t0:t0 + LT], ALU.mult, [yB, b("gg")], [OB[2]])
        S_.barrier()

    def swa_phase(l):
        win = din["w_in"][l]
        for j in range(2):
            wa = Alloc(W_BASE)
            wsw_all = [wa([128, 8, 384], BF16) for _ in range(2)]
            wsw = wsw_all[j]
            qkT = wa([128, 3, S], BF16)
            vaug = wa([128, NB, 66], BF16)
            sqv2 = [wa([128, 320], F32) for _ in range(2)]
            ssq2 = [wa([128, 8], F32) for _ in range(2)]
            tmpq2 = [wa([128, 320], F32) for _ in range(2)]
            qn = [wa([128, 384], BF16) for _ in range(2)]
            pex = [wa([128, 512], BF16) for _ in range(4)]
            oat = [wa([128, 256], BF16) for _ in range(2)]
            den = wa([128, 8], F32)
            wB = b(f"wsw{j}")
            rr = lambda a: a.rearrange("(kc p) n -> p kc n", p=128)
            if j == 0:
                for j2 in range(2):
                    LD("pool", wsw_all[j2][:, :, 0:256], rr(win[:, O_QA + j2 * 256:O_QA + (j2 + 1) * 256]), [b(f"wsw{j2}")])
                    LD("pool", wsw_all[j2][:, :, 256:320], rr(win[:, O_KA + j2 * 64:O_KA + (j2 + 1) * 64]), [b(f"wsw{j2}")])
                    LD("pool", wsw_all[j2][:, :, 320:384], rr(win[:, O_VA + j2 * 64:O_VA + (j2 + 1) * 64]), [b(f"wsw{j2}")])
            vB, qkB = b("vaug"), b("qkT")
            MS("pool", vaug[:, :, 64:65], 1.0, [vB])
            for tb in range(NB):
                bk = tb % 4
                par = tb % 2
                sqv, ssq, tmpq = sqv2[par], ssq2[par], tmpq2[par]
                sqB, ssB_, tqB = b(f"sqv{par}"), b(f"ssq{par}"), b(f"tmpq{par}")
                tbk = 7 if par == 0 else 6
                tbv = psum[:, tbk, :].bitcast(BF16)
                for kc in range(8):
                    MM(psum[:, bk, 0:384], hT[:, kc, tb * 128:(tb + 1) * 128], wsw[:, kc, :], kc == 0, kc == 7, [HB, wB], [PB[bk]])
                ACT(sqv, psum[:, bk, 0:320], AF.Square, [PB[bk]], [sqB])
                S_.op("dve", lambda e, sqv=sqv, ssq=ssq: e.tensor_reduce(out=ssq[:, 0:5], in_=sqv.rearrange("p (h d) -> p h d", d=64), axis=AX.X, op=ALU.add),
                      [sqB], [ssB_])
                ACT(ssq[:, 0:5], ssq[:, 0:5], AF.Ln, [ssB_], [ssB_], scale=1.0 / HD, bias=EPS)
                ACT(ssq[:, 0:5], ssq[:, 0:5], AF.Exp, [ssB_], [ssB_], scale=-0.5)
                TT("dve", tmpq.rearrange("p (h d) -> p h d", d=64), psum[:, bk, 0:320].rearrange("p (h d) -> p h d", d=64),
                   ssq[:, 0:5].unsqueeze(2).to_broadcast([128, 5, 64]), ALU.mult, [PB[bk], ssB_], [tqB])
                qnB = b(f"qn{par}")
                TT("dve", qn[par][:, 0:256].rearrange("p (h d) -> p h d", d=64), tmpq[:, 0:256].rearrange("p (h d) -> p h d", d=64),
                   qg8.unsqueeze(1).to_broadcast([128, 4, 64]), ALU.mult, [tqB, SMALL[0]], [qnB])
                TT("dve", qn[par][:, 256:384].rearrange("p (h d) -> p h d", d=64), tmpq[:, 256:320].unsqueeze(1).to_broadcast([128, 2, 64]),
                   kg.unsqueeze(1).to_broadcast([128, 2, 64]), ALU.mult, [tqB, SMALL[1]], [qnB])
                S_.op("act", lambda e, tb=tb, bk=bk: e.copy(out=vaug[:, tb, 0:64], in_=psum[:, bk, 320:384]), [PB[bk]], [vB])
                for s3 in range(3):
                    TR(tbv[:, s3 * 128:(s3 + 1) * 128], qn[par][:, s3 * 128:(s3 + 1) * 128], identb, [qnB], [PB[tbk]])
                CP("dve", qkT[:, :, tb * 128:(tb + 1) * 128], tbv[:, 0:384].rearrange("p (c t) -> p c t", t=128), [PB[tbk]], [qkB])
            pi_ = 0
            if dbg == "swa1":
                S_.barrier()
                return
            for tb in range(NB):
                kbs = [tb - 1, tb] if tb > 0 else [tb]
                pxs = []
                for kb in kbs:
                    bkp = 2 + 2 * (pi_ % 2)
                    px = pex[pi_ % 4]
                    pB = b(f"pex{pi_ % 4}")
                    pi_ += 1
                    for hh in range(2):
                        MM(psum[:, bkp + hh, 0:256].rearrange("p (a t) -> p a t", t=128), qkT[hh * 64:(hh + 1) * 64, 2, kb * 128:(kb + 1) * 128],
                           qkT[hh * 64:(hh + 1) * 64, 0:2, tb * 128:(tb + 1) * 128], True, True, [qkB], [PB[bkp + hh]])
                    for hh in range(2):
                        MM(psum[:, bkp + hh, 0:256], identb, (mD if kb == tb else mP)[:, 0:256], False, True, [cB], [PB[bkp + hh]], skip=True)
                    ACT(px.rearrange("p (a n) -> p a n", n=256), psum[:, bkp:bkp + 2, 0:256], AF.Exp, [PB[bkp], PB[bkp + 1]], [pB])
                    pxs.append((px, pB, kb))
                par = tb % 2
                if dbg == "swa2":
                    continue
                pvb = 6 if par == 0 else 0
                tbk = 7 if par == 0 else 1
                tbv = psum[:, tbk, :].bitcast(BF16)
                dn = den[:, 4 * par:4 * par + 4]
                pv = psum[:, pvb, 0:264].rearrange("p (s d) -> p s d", d=66)
                for slot in range(4):
                    for n_, (px, pB, kb) in enumerate(pxs):
                        MM(pv[:, slot, 0:65], px[:, slot * 128:(slot + 1) * 128], vaug[:, kb, 0:65], n_ == 0, n_ == len(pxs) - 1,
                           [pB, vB], [PB[pvb]])
                dB = b(f"den{par}")
                for hh in range(2):
                    for cc in range(2):
                        s_ = hh * 2 + cc
                        hd = 4 * j + 2 * cc + hh
                        TT("dve", dn[:, s_:s_ + 1], pv[:, s_, 64:65], esk[:, hd:hd + 1], ALU.add, [PB[pvb], SMALL[2]], [dB])
                S_.op("dve", lambda e, dn=dn: e.reciprocal(out=dn, in_=dn), [dB], [dB])
                oB = b(f"oat{par}")
                for hh in range(2):
                    TT("dve", oat[par].rearrange("p (cc hh d) -> p hh cc d", hh=2, d=64)[:, hh], pv[:, 2 * hh:2 * hh + 2, 0:64],
                       dn[:, 2 * hh:2 * hh + 2].unsqueeze(2).to_broadcast([128, 2, 64]), ALU.mult, [PB[pvb], dB], [oB])
                for cc in range(2):
                    TR(tbv[:, cc * 128:(cc + 1) * 128], oat[par][:, cc * 128:(cc + 1) * 128], identb, [oB], [PB[tbk]])
                S_.op("act", lambda e, tb=tb, j=j, tbv=tbv: e.copy(out=oT[0][:, 2 * j:2 * j + 2, tb * 128:(tb + 1) * 128],
                                                                   in_=tbv[:, 0:256].rearrange("p (c t) -> p c t", t=128)), [PB[tbk]], [OB[0]])
            S_.barrier()
            if dbg == "swa3":
                return

    def sb_phase(l):
        win = din["w_in"][l]
        wa = Alloc(W_BASE)
        wsb = wa([128, 8, 3, 128], BF16)
        qT = wa([128, S], BF16)
        kT = wa([128, S], BF16)
        vtok = wa([128, NB, 128], BF16)
        spf = [wa([128, 512], F32) for _ in range(3)]
        spb = [wa([128, 512], BF16) for _ in range(3)]
        Wt = [wa([128, 512], BF16) for _ in range(3)]
        Ssum = wa([128, 512], F32)
        Ssb = [wa([128, 512], BF16) for _ in range(2)]
        rr = lambda a: a.rearrange("(kc p) n -> p kc n", p=128)
        wB = b("wsb")

        def load_w(c):
            for i3, off in enumerate((O_QB, O_KB, O_VB)):
                LD("pool", wsb[:, :, i3, :], rr(win[:, off + c * 128:off + (c + 1) * 128]), [wB])

        load_w(0)
        zi = 0
        for c in range(4):
            qB, kB_, vB = b("qT"), b("kT"), b("vtok")
            for tg in range(NG):
                for which in range(2):
                    bk = zi % 4
                    zi += 1
                    for kc in range(8):
                        MM(bank(bk), wsb[:, kc, which, :], hT[:, kc, tg * 512:(tg + 1) * 512], kc == 0, kc == 7, [wB, HB], [PB[bk]])
                    if which == 0:
                        ACT(qT[:, tg * 512:(tg + 1) * 512], bank(bk), AF.Copy, [PB[bk]], [qB], scale=0.125)
                    else:
                        CP("dve", kT[:, tg * 512:(tg + 1) * 512], bank(bk), [PB[bk]], [kB_])
            for t4 in range(NB // 4):
                bk = zi % 4
                zi += 1
                for t_ in range(4):
                    tb = t4 * 4 + t_
                    for kc in range(8):
                        MM(psum[:, bk, t_ * 128:(t_ + 1) * 128], hT[:, kc, tb * 128:(tb + 1) * 128], wsb[:, kc, 2, :], kc == 0, kc == 7,
                           [wB, HB], [PB[bk]])
                CP("dve", vtok[:, t4 * 4:(t4 + 1) * 4, :], bank(bk).rearrange("p (t n) -> p t n", n=128), [PB[bk]], [vB])
            if c < 3:
                load_w(c + 1)
            for hh in range(2):
                p0, p1 = hh * 64, (hh + 1) * 64
                for qc in range(NG):
                    kbs = list(range(4 * qc + 3, -1, -1))
                    n = len(kbs)
                    pob = 4 + ((hh * NG + qc) % 2)
                    st = {}

                    def c0of(i):
                        k_ = kbs[i] - 4 * qc
                        return 128 * k_ if k_ > 0 else 0

                    def stage1(i):
                        kb = kbs[i]
                        bk = zi_base[0] % 4
                        zi_base[0] += 1
                        st[i] = bk
                        diag = kb >= 4 * qc
                        c0 = c0of(i)
                        sB, bB = b(f"spf{i % 3}"), b(f"spb{i % 3}")
                        MM(psum[:, bk, c0:512], kT[p0:p1, kb * 128:(kb + 1) * 128], qT[p0:p1, qc * 512 + c0:(qc + 1) * 512], True, True, [kB_, qB], [PB[bk]])
                        if diag:
                            MM(psum[:, bk, c0:512], identb, sbm[:, kb - 4 * qc, c0:512], False, True, [cB], [PB[bk]], skip=True)
                        ACT(spf[i % 3][:, c0:512], psum[:, bk, c0:512], AF.Exp, [PB[bk]], [sB])
                        ACT(spb[i % 3][:, c0:512], spf[i % 3][:, c0:512], AF.Ln, [sB], [bB], bias=1.0)

                    def stage2(i):
                        bk = st[i]
                        c0 = c0of(i)
                        bB = b(f"spb{i % 3}")
                        wtB = b(f"Wt{i % 3}")
                        MM(psum[:, bk, c0:512], Uneg, spb[i % 3][:, c0:512], False, True, [bB], [PB[bk]], skip=True)
                        if i > 0:
                            MM(psum[:, bk, c0:512], onesneg, Ssb[i % 2][:, c0:512], False, True, [b(f"Ssb{i % 2}")], [PB[bk]], skip=True)
                        if i == 0 and c0 > 0:
                            MS("pool", Wt[i % 3][:, 0:c0], 0.0, [wtB])
                        ACT(Wt[i % 3][:, c0:512], psum[:, bk, c0:512], AF.Exp, [PB[bk]], [wtB])
                        if i < n - 1:
                            c1 = c0of(i + 1)
                            if i == 0:
                                if c0 > 0:
                                    MS("pool", Ssum[:, 0:c0], 0.0, [b("Ssum")])
                                CP("dve", Ssum[:, c0:512], spb[i % 3][:, c0:512], [bB], [b("Ssum")])
                            else:
                                TT("dve", Ssum[:, c0:512], Ssum[:, c0:512], spb[i % 3][:, c0:512], ALU.add, [b("Ssum"), bB], [b("Ssum")])
                            CP("dve", Ssb[(i + 1) % 2][:, c1:512], Ssum[:, c1:512], [b("Ssum")], [b(f"Ssb{(i + 1) % 2}")])

                    def stage3(i):
                        kb = kbs[i]
                        c0 = 0 if i == 0 else c0of(i)
                        MM(psum[p0:p1, pob, c0:512], vtok[:, kb, p0:p1], Wt[i % 3][:, c0:512], i == 0, i == n - 1, [vB, b(f"Wt{i % 3}")], [PB[pob]])

                    zi_base = [zi]
                    for step in range(n + 2):
                        if step < n:
                            stage1(step)
                        if 0 <= step - 1 < n:
                            stage2(step - 1)
                        if 0 <= step - 2 < n:
                            stage3(step - 2)
                    zi = zi_base[0]
                    S_.op("act", lambda e, p0=p0, p1=p1, pob=pob, c=c, qc=qc: e.copy(
                        out=oT[1][p0:p1, c, qc * 512:(qc + 1) * 512], in_=psum[p0:p1, pob, :]), [PB[pob]], [OB[1]])
        S_.barrier()

    def merge_phase(l):
        win = din["w_in"][l]
        wps = [din["w_proj_a"][l], din["w_proj_b"][l], din["w_proj_c"][l]]
        wa = Alloc(W_BASE)
        gw = [wa([128, 8, 3, 128], BF16) for _ in range(2)]
        pw = [wa([128, 4, 3, 128], BF16) for _ in range(2)]
        ow = [wa([128, D], BF16) for _ in range(2)]
        sg = [wa([128, 512], F32) for _ in range(3)]
        tt = [wa([128, 512], F32) for _ in range(2)]
        mT = [wa([128, 512], BF16) for _ in range(2)]
        rr = lambda a: a.rearrange("(kc p) n -> p kc n", p=128)

        def load_gp(m):
            wB = b(f"mwg{m % 2}")
            for i in range(3):
                LD("pool", gw[m % 2][:, :, i, :], rr(win[:, O_GT + i * D + m * 128:O_GT + i * D + (m + 1) * 128]), [wB])
                LD("pool", pw[m % 2][:, :, i, :], rr(wps[i][:, m * 128:(m + 1) * 128]), [wB])

        def load_o(m):
            LD("pool", ow[m % 2], din["w_out"][l][m * 128:(m + 1) * 128, :], [b(f"mwo{m % 2}")])

        cnt = {"gi": 0, "oi": 0}

        def GP(m, tg, sp_):
            wB = b(f"mwg{m % 2}")
            tsl = slice(tg * 512, (tg + 1) * 512)
            for i in range(3):
                bk = cnt["gi"] % 4
                cnt["gi"] += 1
                for kc in range(8):
                    MM(bank(bk), gw[m % 2][:, kc, i, :], hT[:, kc, tsl], kc == 0, kc == 7, [wB, HB], [PB[bk]])
                ACT(sg[i], bank(bk), AF.Sigmoid, [PB[bk], SMALL[11]], [b(f"sg{i}")], bias=bg[:, i * 8 + m:i * 8 + m + 1], scale=1.0)
            mB = b(f"mT{sp_ % 2}")
            for i in range(3):
                bk = cnt["gi"] % 4
                cnt["gi"] += 1
                for kc in range(4):
                    MM(bank(bk), pw[m % 2][:, kc, i, :], oT[i][:, kc, tsl], kc == 0, kc == 3, [wB, OB[i]], [PB[bk]])
                if i == 0:
                    TT("dve", tt[0], sg[0], bank(bk), ALU.mult, [b("sg0"), PB[bk]], [b("tt0")])
                elif i == 1:
                    TT("dve", tt[1], sg[1], bank(bk), ALU.mult, [b("sg1"), PB[bk]], [b("tt1")])
                    TT("pool", tt[0], tt[0], tt[1], ALU.add, [b("tt0"), b("tt1")], [b("tt0")])
                else:
                    TT("dve", tt[1], sg[2], bank(bk), ALU.mult, [b("sg2"), PB[bk]], [b("tt1")])
                    TT("pool", mT[sp_ % 2], tt[0], tt[1], ALU.add, [b("tt0"), b("tt1")], [mB])

        def OUT(m, tg, sp_):
            mB = b(f"mT{sp_ % 2}")
            for t_ in range(4):
                tb = tg * 4 + t_
                for cg in range(2):
                    bk = 4 + (cnt["oi"] % 4)
                    cnt["oi"] += 1
                    MM(bank(bk), mT[sp_ % 2][:, t_ * 128:(t_ + 1) * 128], ow[m % 2][:, cg * 512:(cg + 1) * 512], True, True,
                       [mB, b(f"mwo{m % 2}")], [PB[bk]])
                    xs = xres[:, tb, cg * 512:(cg + 1) * 512]
                    TT("dve", xs, xs, bank(bk), ALU.add, [XB[tb][cg], PB[bk]], [XB[tb][cg]])

        load_gp(0)
        load_o(0)
        prev = None
        for m in range(8):
            if m < 7:
                load_gp(m + 1)
            for tg in range(NG):
                GP(m, tg, m * NG + tg)
                if prev is not None:
                    OUT(*prev)
                if tg == 0 and m < 7:
                    load_o(m + 1)
                prev = (m, tg, m * NG + tg)
        OUT(*prev)
        S_.barrier()

    class FFN:
        def __init__(self, units, base):
            wa = Alloc(base)
            self.wg = [wa([128, 8, 512], BF16) for _ in range(2)]
            self.wu = [wa([128, 8, 512], BF16) for _ in range(2)]
            self.wd = [wa([128, 4, D], BF16) for _ in range(2)]
            self.sg = [wa([128, 512], F32) for _ in range(2)]
            self.act = [wa([128, 4, 512], BF16) for _ in range(2)]
            self.end = wa.off
            NFG = DFF // 512
            self.items = [(u, fg) for u in units for fg in range(NFG)]

        def load(self, n):
            (wg_d, wu_d, wd_d, _e), fg = self.items[n]
            rr = lambda a: a.rearrange("(kc p) n -> p kc n", p=128)
            wB = b(f"fw{n % 2}")
            LD("pool", self.wg[n % 2], rr(wg_d[:, fg * 512:(fg + 1) * 512]), [wB])
            LD("pool", self.wu[n % 2], rr(wu_d[:, fg * 512:(fg + 1) * 512]), [wB])
            LD("pool", self.wd[n % 2], rr(wd_d[fg * 512:(fg + 1) * 512, :]), [wB])

        def run(self):
            gi = oi = ai = 0
            for n, ((wg_d, wu_d, wd_d, e_idx), fg) in enumerate(self.items):
                if n + 1 < len(self.items):
                    self.load(n + 1)
                wB = b(f"fw{n % 2}")
                wg, wu, wd = self.wg[n % 2], self.wu[n % 2], self.wd[n % 2]
                for tg in range(NG):
                    tsl = slice(tg * 512, (tg + 1) * 512)
                    aB = b(f"act{ai % 2}")
                    a_ = self.act[ai % 2]
                    ai += 1
                    for fc in range(4):
                        bg_ = gi % 4
                        gi += 1
                        bu_ = gi % 4
                        gi += 1
                        sB = b(f"fsg{fc % 2}")
                        for kc in range(8):
                            MM(bank(bg_), wg[:, kc, fc * 128:(fc + 1) * 128], hT[:, kc, tsl], kc == 0, kc == 7, [wB, HB], [PB[bg_]])
                        for kc in range(8):
                            MM(bank(bu_), wu[:, kc, fc * 128:(fc + 1) * 128], hT[:, kc, tsl], kc == 0, kc == 7, [wB, HB], [PB[bu_]])
                        ACT(self.sg[fc % 2], bank(bg_), AF.Silu, [PB[bg_]], [sB])
                        TT("dve", a_[:, fc, :], self.sg[fc % 2], bank(bu_), ALU.mult, [sB, PB[bu_]], [aB])
                    for t_ in range(4):
                        tb = tg * 4 + t_
                        for cg in range(2):
                            bk = 4 + (oi % 4)
                            oi += 1
                            for fc in range(4):
                                MM(bank(bk), a_[:, fc, t_ * 128:(t_ + 1) * 128], wd[:, fc, cg * 512:(cg + 1) * 512], fc == 0, fc == 3,
                                   [aB, wB], [PB[bk]])
                            xs = xres[:, tb, cg * 512:(cg + 1) * 512]
                            if e_idx is None:
                                TT("dve", xs, xs, bank(bk), ALU.add, [XB[tb][cg], PB[bk]], [XB[tb][cg]])
                            else:
                                STT(xs, bank(bk), comb[:, tb, e_idx:e_idx + 1], xs, ALU.mult, ALU.add,
                                    [XB[tb][cg], PB[bk], b("comb")], [XB[tb][cg]])
            S_.barrier()

    oB = b("out")

    def dump(br_):
        stg = view(W_BASE, [128, 4 * S], F32)
        CP("dve", stg, oT[br_].rearrange("p c s -> p (c s)"), [OB[br_]], [b("stg")])
        S_.barrier()
        LD("sp", out[0].rearrange("(p a) d -> p (a d)", p=128)[:, 0:4 * S], stg, [b("dumped")])
        S_.barrier()

    for l in range(DEPTH):
        set_layer(l)
        load_layer_consts(l)
    for seq in range(NSEQ):
        xv = din["x"][seq].rearrange("(tb p) d -> p tb d", p=128)
        for q4 in range(0, NB, 4):
            LD("sp", xres[:, q4:q4 + 4, :], xv[:, q4:q4 + 4, :], [XB[tb][cg] for tb in range(q4, q4 + 4) for cg in range(2)])
        for l in range(DEPTH):
            set_layer(l)
            norm_phase(din["attn_norm"][l], W_BASE)
            if dbg == "hT":
                break
            lru_phase(l)
            if dbg == "lru":
                dump(2)
                break
            swa_phase(l)
            if dbg in ("swa", "swa1", "swa2", "swa3"):
                dump(0)
                break
            sb_phase(l)
            if dbg == "sb":
                dump(1)
                break
            merge_phase(l)
            if dbg == "mixer":
                break
            j = l // 2
            if l % 2 == 0:
                units = [(din["w_ffn_gate"][j], din["w_ffn_up"][j], din["w_ffn_down"][j], None)]
            else:
                units = [(din["w_exp_gate"][j, e], din["w_exp_up"][j, e], din["w_exp_down"][j, e], e) for e in range(NE)]
            ffn = FFN(units, O_BASE)
            ffn.load(0)
            norm_phase(din["ffn_norm"][l], ffn.end, router_w=(din["w_router"][j] if l % 2 else None))
            ffn.run()
        ov = out[seq].rearrange("(tb p) d -> p tb d", p=128)
        for q4 in range(0, NB, 4):
            if dbg in ("lru", "swa", "sb", "swa1", "swa2", "swa3"):
                break
            S_.dma("sp", ov[:, q4:q4 + 4, :], xres[:, q4:q4 + 4, :], [XB[tb][cg] for tb in range(q4, q4 + 4) for cg in range(2)], [oB])
        S_.barrier()
    S_.barrier()
    S_.emit()
    return nc


_NC_CACHE = {}


def kernel(**inputs):
    x = np.ascontiguousarray(inputs["x"], dtype=np.float32)
    BATCH = x.shape[0]
    per = BATCH // NCORES
    if "nc" not in _NC_CACHE:
        _NC_CACHE["nc"] = build(S=x.shape[1], NSEQ=per, DEPTH=inputs["w_in"].shape[0])
    nc = _NC_CACHE["nc"]
    shared = {k: np.ascontiguousarray(v, dtype=np.float32) for k, v in inputs.items() if k != "x"}
    in_maps = []
    for i in range(NCORES):
        m = dict(shared)
        m["x"] = np.ascontiguousarray(x[i * per:(i + 1) * per])
        in_maps.append(m)
    res = run_bass_kernel_spmd(nc, in_maps, core_ids=list(range(NCORES)))
    return np.concatenate([np.asarray(r["out"]) for r in res.results], axis=0).astype(np.float32)
```

```python
import numpy as np
import concourse.bass as bass
import concourse.mybir as mybir
from concourse.bass_utils import run_bass_kernel_spmd

F32 = mybir.dt.float32
BF16 = mybir.dt.bfloat16
AF = mybir.ActivationFunctionType
ALU = mybir.AluOpType
AX = mybir.AxisListType

D = 1024
HD = 64
INC = 6400
DFF = 3584
NE = 8
EPS = 1e-6
O_QA, O_KA, O_VA, O_QB, O_KB, O_VB, O_XC, O_GC, O_GT = 0, 512, 640, 768, 1280, 1792, 2304, 2816, 3328
NCORES = 8
P_LRU, P_SWA, P_SB, P_MG = 0, 8192, 14336, 26624


class Buf:
    __slots__ = ("name", "w", "rs")

    def __init__(self, name):
        self.name = name
        self.w = None
        self.rs = {}


class Sched:
    SEM_LIMIT = 30000
    NDMA = 6

    def __init__(self, nc):
        self.nc = nc
        self.names = ["pe", "act", "dve", "pool", "sp"]
        self.lists = {k: [] for k in self.names}
        self.seen = {k: {} for k in self.names}
        self.cur = {}
        self.sems = {}
        self.last = {}
        self.dma_slots = {}
        self.dma_rr = {k: 0 for k in self.names}
        self.ctxs = []

    def _newsem(self, name):
        cm = self.nc.semaphore(name)
        self.sems[name] = cm.__enter__()
        self.ctxs.append(cm)
        return name

    def _eng_event(self, eng):
        c = self.cur.get(eng)
        if c is None or c[1] >= self.SEM_LIMIT:
            ep = 0 if c is None else c[2] + 1
            c = [self._newsem(f"s_{eng}_{ep}"), 0, ep]
            self.cur[eng] = c
        c[1] += 1
        self.last[c[0]] = c[1]
        return (c[0], c[1])

    def _need(self, eng, evs):
        out = {}
        for ev in evs:
            if ev is None:
                continue
            k, v = ev
            if self.seen[eng].get(k, 0) >= v:
                continue
            if out.get(k, 0) < v:
                out[k] = v
        for k, v in out.items():
            self.seen[eng][k] = v
        return list(out.items())

    def _deps(self, eng, reads, writes):
        c = self.cur.get(eng)
        own = c[0] if c else None
        evs = []
        for b in reads:
            evs.append(b.w)
        for b in writes:
            evs.append(b.w)
            evs.extend(b.rs.items())
        if eng == "pe":
            evs = [e for e in evs if e is not None and e[0] != own]
        return self._need(eng, evs)

    def _commit(self, ev, reads, writes):
        for b in reads:
            if b.rs.get(ev[0], 0) < ev[1]:
                b.rs[ev[0]] = ev[1]
        for b in writes:
            b.w = ev
            b.rs = {}

    def op(self, eng, fn, reads=(), writes=()):
        waits = self._deps(eng, reads, writes)
        ev = self._eng_event(eng)
        self.lists[eng].append((waits, fn, ev, 1))
        self._commit(ev, reads, writes)
        return ev

    def dma(self, q, out, in_, reads=(), writes=(), **kw):
        slots = self.dma_slots.setdefault(q, [])
        if len(slots) < self.NDMA:
            slots.append([self._newsem(f"d_{q}_{len(slots)}"), 0])
        slot = slots[self.dma_rr[q] % self.NDMA]
        self.dma_rr[q] += 1
        waits = self._deps(q, reads, writes)
        if slot[1] > 0:
            waits += self._need(q, [(slot[0], slot[1])])
        slot[1] += 16
        ev = (slot[0], slot[1])
        self.last[slot[0]] = slot[1]
        fn = lambda e: e.dma_start(out=out, in_=in_, **kw)
        self.lists[q].append((waits, fn, ev, 16))
        self._commit(ev, reads, writes)
        return ev

    def barrier(self):
        evs = list(self.last.items())
        for eng in self.names:
            waits = self._need(eng, evs)
            if waits:
                self.lists[eng].append((waits, None, None, 0))

    def emit(self):
        nc = self.nc
        engs = {"pe": "tensor", "act": "scalar", "dve": "vector", "pool": "gpsimd", "sp": "sync"}
        with nc.Block() as block:
            def mk(name):
                def body(e):
                    for waits, fn, ev, inc in self.lists[name]:
                        for k, v in waits:
                            e.wait_ge(self.sems[k], v)
                        if fn is not None:
                            fn(e).then_inc(self.sems[ev[0]], inc)
                return body
            for name in self.names:
                getattr(block, engs[name])(mk(name))
        for cm in reversed(self.ctxs):
            cm.__exit__(None, None, None)


def build(S=2048, NSEQ=2, DEPTH=2, dbg=None):
    NB = S // 128
    NG = S // 512
    LT = 512
    nc = bass.Bass("TRN2", target_bir_lowering=False)
    din = {}

    def dram_in(name, shape):
        din[name] = nc.dram_tensor(name, list(shape), F32, kind="ExternalInput").ap()

    NDENSE = (DEPTH + 1) // 2
    NMOE = max(DEPTH // 2, 1)
    dram_in("x", [NSEQ, S, D])
    dram_in("attn_norm", [DEPTH, D]); dram_in("w_in_p", [DEPTH, 128, 8 * INC]); dram_in("b_gate", [DEPTH, 3 * D])
    dram_in("q_norm", [DEPTH, HD]); dram_in("k_norm", [DEPTH, HD]); dram_in("sinks", [DEPTH, 8])
    dram_in("conv_w", [DEPTH, 4, 512]); dram_in("conv_b", [DEPTH, 512])
    dram_in("lru_w_r", [DEPTH, 8, 64, 64]); dram_in("lru_b_r", [DEPTH, 512])
    dram_in("lru_w_i", [DEPTH, 8, 64, 64]); dram_in("lru_b_i", [DEPTH, 512]); dram_in("lru_lambda", [DEPTH, 512])
    dram_in("w_pj_p", [DEPTH, 128, 8 * 4 * 3 * 128])
    dram_in("w_out", [DEPTH, D, D]); dram_in("ffn_norm", [DEPTH, D])
    dram_in("w_ffn_gate", [NDENSE, D, DFF]); dram_in("w_ffn_up", [NDENSE, D, DFF]); dram_in("w_ffn_down", [NDENSE, DFF, D])
    dram_in("w_router", [NMOE, D, NE]); dram_in("w_exp_gate", [NMOE, NE, D, DFF])
    dram_in("w_exp_up", [NMOE, NE, D, DFF]); dram_in("w_exp_down", [NMOE, NE, DFF, D])
    out = nc.dram_tensor("out", [NSEQ, S, D], F32, kind="ExternalOutput").ap()

    S_ = Sched(nc)
    ARENA = 208000
    arena = nc.alloc_sbuf_tensor("arena", [128, ARENA // 4], F32).ap()
    psum = nc.alloc_psum_tensor("psum", [128, 8, 512], F32).ap()

    def bank(i):
        return psum[:, i, :]

    def view(off, shape, dt):
        n = int(np.prod(shape[1:]))
        sz = 4 if dt == F32 else 2
        assert off % 4 == 0 and (n * sz) % 4 == 0, (off, shape)
        assert off + n * sz <= ARENA, ("arena overflow", off, shape)
        w = arena[:, off // 4:(off + n * sz) // 4]
        v = w if dt == F32 else w.bitcast(dt)
        if len(shape) == 3:
            v = v.rearrange("p (a b) -> p a b", b=shape[2])
        elif len(shape) == 4:
            v = v.rearrange("p (a b c) -> p a b c", b=shape[2], c=shape[3])
        return v

    class Alloc:
        def __init__(self, base):
            self.off = base

        def __call__(self, shape, dt):
            n = int(np.prod(shape[1:])) * (4 if dt == F32 else 2)
            n = (n + 31) // 32 * 32
            v = view(self.off, shape, dt)
            self.off += n
            return v

    pa = Alloc(0)
    identb = pa([128, 128], BF16)
    identf = pa([128, 128], F32)
    onesb = pa([128, 128], BF16)
    Uneg = pa([128, 128], BF16)
    onesneg = pa([128, 128], BF16)
    mD = pa([128, 512], BF16)
    mP = pa([128, 512], BF16)
    sbm = pa([128, 4, 512], BF16)
    gt = pa([128, D], F32)
    LC = []
    for _l in range(DEPTH):
        LC.append(dict(qg8=pa([128, HD], F32), kg=pa([128, HD], F32), esk=pa([128, 8], F32), cw=pa([128, 4, 4], F32),
                       cb=pa([128, 4], F32), br=pa([128, 4], F32), bi=pa([128, 4], F32), clam=pa([128, 4], F32),
                       bg=pa([128, 24], F32), wbd=pa([128, 4, 2, 128], BF16)))
    qg8 = kg = esk = cw = cb = br = bi = clam = bg = wbd = None

    def set_layer(l):
        nonlocal qg8, kg, esk, cw, cb, br, bi, clam, bg, wbd
        d_ = LC[l]
        qg8, kg, esk, cw, cb, br, bi, clam, bg, wbd = (d_["qg8"], d_["kg"], d_["esk"], d_["cw"], d_["cb"], d_["br"], d_["bi"],
                                                       d_["clam"], d_["bg"], d_["wbd"])
    comb = pa([128, NB, NE], F32)
    hprev = pa([128, 2], F32)
    xres = pa([128, NB, D], F32)
    hT = pa([128, 8, S], BF16)
    O_BASE = pa.off
    oT = [pa([128, 4, S], BF16) for _ in range(3)]
    W_BASE = pa.off

    B = {}

    def b(name):
        if name not in B:
            B[name] = Buf(name)
        return B[name]

    PB = [b(f"bank{i}") for i in range(8)]

    def MM(out, lhsT, rhs, start, stop, reads, writes, skip=False):
        S_.op("pe", lambda e: e.matmul(out=out, lhsT=lhsT, rhs=rhs, start=start, stop=stop, skip_group_check=skip), reads, writes)

    def TR(out, in_, ident, reads, writes):
        S_.op("pe", lambda e: e.transpose(out=out, in_=in_, identity=ident), reads, writes)

    def ACT(out, in_, func, reads, writes, **kw):
        S_.op("act", lambda e: e.activation(out=out, in_=in_, func=func, **kw), reads, writes)

    def TT(eng, out, in0, in1, op, reads, writes):
        S_.op(eng, lambda e: e.tensor_tensor(out=out, in0=in0, in1=in1, op=op), reads, writes)

    def TS(eng, out, in0, s1, s2, op0, op1, reads, writes):
        S_.op(eng, lambda e: e.tensor_scalar(out=out, in0=in0, scalar1=s1, scalar2=s2, op0=op0, op1=op1), reads, writes)

    def STT(out, in0, scalar, in1, op0, op1, reads, writes):
        S_.op("dve", lambda e: e.scalar_tensor_tensor(out=out, in0=in0, scalar=scalar, in1=in1, op0=op0, op1=op1), reads, writes)

    def CP(eng, out, in_, reads, writes):
        S_.op(eng, lambda e: e.tensor_copy(out=out, in_=in_), reads, writes)

    def MS(eng, ap, val, writes):
        S_.op(eng, lambda e: e.memset(ap, val), (), writes)

    def ASEL(out, in_, pattern, cmp, fill, base, cm, reads, writes):
        S_.op("pool", lambda e: e.affine_select(out=out, in_=in_, pattern=pattern, compare_op=cmp, fill=fill,
                                                base=base, channel_multiplier=cm), reads, writes)

    def LD(q, out_, in_, writes, **kw):
        S_.dma(q, out_, in_, (), writes, **kw)

    bank7b = psum[:, 7, :].bitcast(BF16)

    tmpc = view(W_BASE, [128, 512], F32)
    cB = b("consts")
    MS("pool", identf, 0.0, [cB])
    ASEL(identf, identf, [[-1, 128]], ALU.not_equal, 1.0, 0, 1, [cB], [cB])
    CP("dve", identb, identf, [cB], [cB])
    MS("pool", onesb, 1.0, [cB])
    MS("pool", onesneg, -1.0, [cB])
    MS("pool", tmpc[:, 0:128], -1.0, [cB])
    ASEL(tmpc[:, 0:128], tmpc[:, 0:128], [[-1, 128]], ALU.is_ge, 0.0, 0, 1, [cB], [cB])
    CP("dve", Uneg, tmpc[:, 0:128], [cB], [cB])
    MS("pool", tmpc, 1.0, [cB])
    ASEL(tmpc.rearrange("p (a t) -> p a t", t=128), tmpc.rearrange("p (a t) -> p a t", t=128), [[0, 4], [1, 128]],
         ALU.is_ge, 0.0, 0, -1, [cB], [cB])
    TS("dve", tmpc, tmpc, 30000.0, -30000.0, ALU.mult, ALU.add, [cB], [cB])
    CP("dve", mD, tmpc, [cB], [cB])
    MS("pool", tmpc, 1.0, [cB])
    ASEL(tmpc.rearrange("p (a t) -> p a t", t=128), tmpc.rearrange("p (a t) -> p a t", t=128), [[0, 4], [-1, 128]],
         ALU.is_gt, 0.0, 0, 1, [cB], [cB])
    TS("dve", tmpc, tmpc, 30000.0, -30000.0, ALU.mult, ALU.add, [cB], [cB])
    CP("dve", mP, tmpc, [cB], [cB])
    for k in range(4):
        MS("pool", tmpc, 1.0, [cB])
        ASEL(tmpc, tmpc, [[1, 512]], ALU.is_gt, 0.0, -128 * k, -1, [cB], [cB])
        TS("dve", tmpc, tmpc, 30000.0, -30000.0, ALU.mult, ALU.add, [cB], [cB])
        CP("dve", sbm[:, k, :], tmpc, [cB], [cB])
    S_.barrier()

    XB = [[b(f"x{tb}_{cg}") for cg in range(2)] for tb in range(NB)]
    HB = b("hT")
    OB = [b(f"oT{i}") for i in range(3)]

    def xbufs(tb):
        return XB[tb]

    SMALL = [b(n) for n in ("s_qg8", "s_kg", "s_esk", "s_cw0", "s_cw1", "s_cw2", "s_cw3", "s_cb", "s_br", "s_bi", "s_clam", "s_bg")]
    WBD = [b(f"s_wbd{i}") for i in range(16)]

    def load_layer_consts(l):
        NCD = dict(allow_slow_non_contiguous=True)
        sq_, sk_, se_, c0, c1, c2, c3, scb, sbr, sbi, scl, sbg = SMALL
        LD("sp", qg8, din["q_norm"][l].partition_broadcast(128), [sq_])
        LD("sp", kg, din["k_norm"][l].partition_broadcast(128), [sk_])
        LD("sp", esk, din["sinks"][l].partition_broadcast(128), [se_])
        for k, cB_ in enumerate((c0, c1, c2, c3)):
            LD("sp", cw[:, :, k], din["conv_w"][l, k].rearrange("(c p) -> p c", p=128), [cB_], **NCD)
        LD("sp", cb, din["conv_b"][l].rearrange("(c p) -> p c", p=128), [scb], **NCD)
        LD("sp", br, din["lru_b_r"][l].rearrange("(c p) -> p c", p=128), [sbr], **NCD)
        LD("sp", bi, din["lru_b_i"][l].rearrange("(c p) -> p c", p=128), [sbi], **NCD)
        LD("sp", clam, din["lru_lambda"][l].rearrange("(c p) -> p c", p=128), [scl], **NCD)
        LD("sp", bg, din["b_gate"][l].rearrange("(c p) -> p c", p=128), [sbg], **NCD)
        MS("pool", wbd, 0.0, WBD)
        n_ = 0
        for c in range(4):
            for hh in range(2):
                LD("pool", wbd[hh * 64:(hh + 1) * 64, c, 0, hh * 64:(hh + 1) * 64], din["lru_w_r"][l, 2 * c + hh], [WBD[n_]])
                LD("pool", wbd[hh * 64:(hh + 1) * 64, c, 1, hh * 64:(hh + 1) * 64], din["lru_w_i"][l, 2 * c + hh], [WBD[n_ + 1]])
                n_ += 2
        TS("dve", qg8, qg8, 0.125, None, ALU.mult, ALU.bypass, [sq_], [sq_])
        ACT(esk, esk, AF.Exp, [se_], [se_])
        ACT(clam, clam, AF.Exp, [scl], [scl], scale=-1.0)
        ACT(clam, clam, AF.Ln, [scl], [scl], bias=1.0)
        TS("dve", clam, clam, -8.0, None, ALU.mult, ALU.bypass, [scl], [scl])
        S_.barrier()

    def norm_phase(gain_dram, base, router_w=None):
        wa = Alloc(base)
        junk = wa([128, D], BF16)
        hb = [wa([128, D], BF16) for _ in range(2)]
        ss = wa([128, 4], F32)
        gB = b("gt")
        LD("sp", gt, gain_dram.partition_broadcast(128), [gB])
        if router_w is not None:
            hf2 = [wa([128, D], F32) for _ in range(2)]
            hTf2 = [wa([128, 8, 128], F32) for _ in range(2)]
            wr = wa([128, 8, NE], F32)
            lg2 = [wa([128, NE], F32) for _ in range(2)]
            t82 = [[wa([128, NE], F32) for _ in range(3)] for _ in range(2)]
            m122 = [wa([128, 4], F32) for _ in range(2)]
            LD("sp", wr, router_w.rearrange("(kc p) e -> p kc e", p=128), [b("wr")])
        for tb in range(NB):
            par = tb % 2
            xr = xres[:, tb, :]
            ssB, hbB = b(f"ss{par}"), b(f"hb{par}")
            ACT(junk, xr, AF.Square, xbufs(tb), [b("junk"), ssB], accum_out=ss[:, par:par + 1])
            ACT(ss[:, par:par + 1], ss[:, par:par + 1], AF.Ln, [ssB], [ssB], scale=1.0 / D, bias=EPS)
            ACT(ss[:, par:par + 1], ss[:, par:par + 1], AF.Exp, [ssB], [ssB], scale=-0.5)
            STT(hb[par], xr, ss[:, par:par + 1], gt, ALU.mult, ALU.mult, xbufs(tb) + [ssB, gB], [hbB])
            tbk = 7 if par == 0 else 3
            tbv = psum[:, tbk, :].bitcast(BF16)
            for c in range(8):
                TR(tbv[:, c * 128:(c + 1) * 128], hb[par][:, c * 128:(c + 1) * 128], identb, [hbB], [PB[tbk]])
            if tb % 2:
                CP("dve", hT[:, :, tb * 128:(tb + 1) * 128], tbv.rearrange("p (c t) -> p c t", t=128), [PB[tbk]], [HB])
            else:
                S_.op("act", lambda e, tb=tb, tbv=tbv: e.copy(out=hT[:, :, tb * 128:(tb + 1) * 128],
                                                              in_=tbv.rearrange("p (c t) -> p c t", t=128)), [PB[tbk]], [HB])
            if router_w is not None:
                hf, hTf, lg, t8, m12 = hf2[par], hTf2[par], lg2[par], t82[par], m122[par]
                tb0 = 5 if par == 0 else 1
                lbk = 4 if par == 0 else 0
                hfB, hTB, lB, mB_ = b(f"hf{par}"), b(f"hTf{par}"), b(f"lg{par}"), b(f"m12{par}")
                t0B, t1B, t2B = b(f"t80{par}"), b(f"t81{par}"), b(f"t82{par}")
                STT(hf, xr, ss[:, par:par + 1], gt, ALU.mult, ALU.mult, xbufs(tb) + [ssB, gB], [hfB])
                for c in range(8):
                    TR(psum[:, tb0 + c // 4, (c % 4) * 128:(c % 4 + 1) * 128], hf[:, c * 128:(c + 1) * 128], identf,
                       [hfB], [PB[tb0 + c // 4]])
                CP("dve", hTf, psum[:, tb0:tb0 + 2, :].rearrange("p a (c t) -> p (a c) t", t=128), [PB[tb0], PB[tb0 + 1]], [hTB])
                for c in range(8):
                    MM(psum[:, lbk, 0:NE], hTf[:, c, :], wr[:, c, :], c == 0, c == 7, [hTB, b("wr")], [PB[lbk]])
                CP("dve", lg, psum[:, lbk, 0:NE], [PB[lbk]], [lB])
                S_.op("dve", lambda e, m12=m12, lg=lg: e.reduce_max(out=m12[:, 0:1], in_=lg, axis=AX.X), [lB], [mB_])
                TS("dve", t8[0], lg, m12[:, 0:1], None, ALU.is_equal, ALU.bypass, [lB, mB_], [t0B])
                STT(t8[1], t8[0], -1e30, lg, ALU.mult, ALU.add, [t0B, lB], [t1B])
                S_.op("dve", lambda e, m12=m12, t8=t8: e.reduce_max(out=m12[:, 1:2], in_=t8[1], axis=AX.X), [t1B], [mB_])
                TS("dve", t8[0], lg, m12[:, 1:2], None, ALU.is_ge, ALU.bypass, [lB, mB_], [t0B])
                TS("dve", m12[:, 2:3], m12[:, 0:1], -1.0, None, ALU.mult, ALU.bypass, [mB_], [mB_])
                ACT(t8[1], lg, AF.Exp, [lB, mB_], [t1B], bias=m12[:, 2:3], scale=1.0)
                TT("dve", t8[2], t8[1], t8[0], ALU.mult, [t1B, t0B], [t2B])
                S_.op("dve", lambda e, m12=m12, t8=t8: e.reduce_sum(out=m12[:, 3:4], in_=t8[2], axis=AX.X), [t2B], [mB_])
                S_.op("dve", lambda e, m12=m12: e.reciprocal(out=m12[:, 3:4], in_=m12[:, 3:4]), [mB_], [mB_])
                TS("dve", comb[:, tb, :], t8[2], m12[:, 3:4], None, ALU.mult, ALU.bypass, [t2B, mB_], [b("comb")])
        S_.barrier()

    def lru_phase(l):
        wa = Alloc(W_BASE)
        xraw = wa([128, S + 4], F32)
        gg = wa([128, S], BF16)
        y2 = [wa([128, LT], F32) for _ in range(2)]
        r2 = [wa([128, LT], F32) for _ in range(2)]
        ii2 = [wa([128, LT], F32) for _ in range(2)]
        t12 = [wa([128, LT], F32) for _ in range(2)]
        xcb2 = [wa([128, LT], BF16) for _ in range(2)]
        ti_ = 0
        wl = [wa([128, 8, 2, 128], BF16) for _ in range(2)]
        win = din["w_in_p"][l]
        MS("pool", xraw[:, 0:3], 0.0, [b("xraw")])
        bi_ = 0
        def load_wl(c):
            LD("pool", wl[c % 2].rearrange("p a b c -> p (a b c)"), win[:, P_LRU + c * 2048:P_LRU + (c + 1) * 2048], [b(f"wl{c % 2}")])

        load_wl(0)
        for c in range(4):
            wB = b(f"wl{c % 2}")
            for tg in range(NG):
                for which in range(2):
                    bk = bi_ % 4
                    bi_ += 1
                    for kc in range(8):
                        MM(bank(bk), wl[c % 2][:, kc, which, :], hT[:, kc, tg * 512:(tg + 1) * 512], kc == 0, kc == 7, [wB, HB], [PB[bk]])
                    if which == 0:
                        S_.op("act", lambda e, bk=bk, tg=tg: e.copy(out=xraw[:, 3 + tg * 512:3 + (tg + 1) * 512], in_=bank(bk)), [PB[bk]], [b("xraw")])
                    else:
                        ACT(gg[:, tg * 512:(tg + 1) * 512], bank(bk), AF.Gelu_apprx_tanh, [PB[bk]], [b("gg")])
            if c < 3:
                load_wl(c + 1)
            for half in range(S // LT):
                t0 = half * LT
                pr_ = ti_ % 2
                ti_ += 1
                y, r, ii, t1, xcb = y2[pr_], r2[pr_], ii2[pr_], t12[pr_], xcb2[pr_]
                yB, rB, iB, tB, xcB = b(f"y{pr_}"), b(f"r{pr_}"), b(f"i{pr_}"), b(f"t1{pr_}"), b(f"xcb{pr_}")
                TS("dve", y, xraw[:, t0:t0 + LT], cw[:, c, 0:1], cb[:, c:c + 1], ALU.mult, ALU.add, [b("xraw")], [yB])
                for k in range(1, 4):
                    STT(y, xraw[:, t0 + k:t0 + k + LT], cw[:, c, k:k + 1], y, ALU.mult, ALU.add, [b("xraw"), yB], [yB])
                CP("pool", xcb, y, [yB], [xcB])
                for sub in range(LT // 512):
                    for which, dst, dB, bias in ((0, r, rB, br), (1, ii, iB, bi)):
                        bk = bi_ % 4
                        bi_ += 1
                        MM(bank(bk), wbd[:, c, which, :], xcb[:, sub * 512:(sub + 1) * 512], True, True, [xcB] + WBD, [PB[bk]])
                        ACT(dst[:, sub * 512:(sub + 1) * 512], bank(bk), AF.Sigmoid, [PB[bk]], [dB], bias=bias[:, c:c + 1], scale=1.0)
                ACT(r, r, AF.Exp, [rB], [rB], scale=clam[:, c:c + 1])
                TT("pool", t1, r, r, ALU.mult, [rB], [tB])
                ACT(t1, t1, AF.Sqrt, [tB], [tB], scale=-1.0, bias=1.0)
                TT("pool", ii, ii, y, ALU.mult, [iB, yB], [iB])
                TT("dve", t1, t1, ii, ALU.mult, [tB, iB], [tB])
                init = 0.0 if half == 0 else hprev[:, 0:1]
                S_.op("dve", lambda e, init=init, y=y, r=r, t1=t1: e.tensor_tensor_scan(out=y, data0=r, data1=t1, initial=init,
                                                                       op0=ALU.mult, op1=ALU.add),
                      [rB, tB, b("hprev")], [yB])
                CP("dve", hprev[:, 0:1], y[:, LT - 1:LT], [yB], [b("hprev")])
                TT("dve", oT[2][:, c, t0:t0 + LT], y, gg[:, t0:t0 + LT], ALU.mult, [yB, b("gg")], [OB[2]])
        S_.barrier()

    def swa_phase(l):
        win = din["w_in_p"][l]
        for j in range(2):
            wa = Alloc(W_BASE)
            wsw_all = [wa([128, 8, 384], BF16) for _ in range(2)]
            wsw = wsw_all[j]
            qkT = wa([128, 3, S], BF16)
            vaug = wa([128, NB, 66], BF16)
            sqv2 = [wa([128, 320], F32) for _ in range(2)]
            ssq2 = [wa([128, 8], F32) for _ in range(2)]
            tmpq2 = [wa([128, 320], F32) for _ in range(2)]
            qn = [wa([128, 384], BF16) for _ in range(2)]
            pex = [wa([128, 512], BF16) for _ in range(4)]
            oat = [wa([128, 256], BF16) for _ in range(2)]
            den = wa([128, 8], F32)
            wB = b(f"wsw{j}")
            rr = lambda a: a.rearrange("(kc p) n -> p kc n", p=128)
            if j == 0:
                for j2 in range(2):
                    LD("pool", wsw_all[j2].rearrange("p a b -> p (a b)"), win[:, P_SWA + j2 * 3072:P_SWA + (j2 + 1) * 3072], [b(f"wsw{j2}")])
            vB, qkB = b("vaug"), b("qkT")
            MS("pool", vaug[:, :, 64:65], 1.0, [vB])
            for tb in range(NB):
                bk = tb % 4
                par = tb % 2
                sqv, ssq, tmpq = sqv2[par], ssq2[par], tmpq2[par]
                sqB, ssB_, tqB = b(f"sqv{par}"), b(f"ssq{par}"), b(f"tmpq{par}")
                tbk = 7 if par == 0 else 6
                tbv = psum[:, tbk, :].bitcast(BF16)
                for kc in range(8):
                    MM(psum[:, bk, 0:384], hT[:, kc, tb * 128:(tb + 1) * 128], wsw[:, kc, :], kc == 0, kc == 7, [HB, wB], [PB[bk]])
                ACT(sqv, psum[:, bk, 0:320], AF.Square, [PB[bk]], [sqB])
                S_.op("dve", lambda e, sqv=sqv, ssq=ssq: e.tensor_reduce(out=ssq[:, 0:5], in_=sqv.rearrange("p (h d) -> p h d", d=64), axis=AX.X, op=ALU.add),
                      [sqB], [ssB_])
                ACT(ssq[:, 0:5], ssq[:, 0:5], AF.Ln, [ssB_], [ssB_], scale=1.0 / HD, bias=EPS)
                ACT(ssq[:, 0:5], ssq[:, 0:5], AF.Exp, [ssB_], [ssB_], scale=-0.5)
                TT("dve", tmpq.rearrange("p (h d) -> p h d", d=64), psum[:, bk, 0:320].rearrange("p (h d) -> p h d", d=64),
                   ssq[:, 0:5].unsqueeze(2).to_broadcast([128, 5, 64]), ALU.mult, [PB[bk], ssB_], [tqB])
                qnB = b(f"qn{par}")
                TT("dve", qn[par][:, 0:256].rearrange("p (h d) -> p h d", d=64), tmpq[:, 0:256].rearrange("p (h d) -> p h d", d=64),
                   qg8.unsqueeze(1).to_broadcast([128, 4, 64]), ALU.mult, [tqB, SMALL[0]], [qnB])
                TT("dve", qn[par][:, 256:384].rearrange("p (h d) -> p h d", d=64), tmpq[:, 256:320].unsqueeze(1).to_broadcast([128, 2, 64]),
                   kg.unsqueeze(1).to_broadcast([128, 2, 64]), ALU.mult, [tqB, SMALL[1]], [qnB])
                S_.op("act", lambda e, tb=tb, bk=bk: e.copy(out=vaug[:, tb, 0:64], in_=psum[:, bk, 320:384]), [PB[bk]], [vB])
                for s3 in range(3):
                    TR(tbv[:, s3 * 128:(s3 + 1) * 128], qn[par][:, s3 * 128:(s3 + 1) * 128], identb, [qnB], [PB[tbk]])
                CP("dve", qkT[:, :, tb * 128:(tb + 1) * 128], tbv[:, 0:384].rearrange("p (c t) -> p c t", t=128), [PB[tbk]], [qkB])
            pi_ = 0
            if dbg == "swa1":
                S_.barrier()
                return
            for tb in range(NB):
                kbs = [tb - 1, tb] if tb > 0 else [tb]
                pxs = []
                for kb in kbs:
                    bkp = 2 + 2 * (pi_ % 2)
                    px = pex[pi_ % 4]
                    pB = b(f"pex{pi_ % 4}")
                    pi_ += 1
                    for hh in range(2):
                        MM(psum[:, bkp + hh, 0:256].rearrange("p (a t) -> p a t", t=128), qkT[hh * 64:(hh + 1) * 64, 2, kb * 128:(kb + 1) * 128],
                           qkT[hh * 64:(hh + 1) * 64, 0:2, tb * 128:(tb + 1) * 128], True, True, [qkB], [PB[bkp + hh]])
                    for hh in range(2):
                        MM(psum[:, bkp + hh, 0:256], identb, (mD if kb == tb else mP)[:, 0:256], False, True, [cB], [PB[bkp + hh]], skip=True)
                    ACT(px.rearrange("p (a n) -> p a n", n=256), psum[:, bkp:bkp + 2, 0:256], AF.Exp, [PB[bkp], PB[bkp + 1]], [pB])
                    pxs.append((px, pB, kb))
                par = tb % 2
                if dbg == "swa2":
                    continue
                pvb = 6 if par == 0 else 0
                tbk = 7 if par == 0 else 1
                tbv = psum[:, tbk, :].bitcast(BF16)
                dn = den[:, 4 * par:4 * par + 4]
                pv = psum[:, pvb, 0:264].rearrange("p (s d) -> p s d", d=66)
                for slot in range(4):
                    for n_, (px, pB, kb) in enumerate(pxs):
                        MM(pv[:, slot, 0:65], px[:, slot * 128:(slot + 1) * 128], vaug[:, kb, 0:65], n_ == 0, n_ == len(pxs) - 1,
                           [pB, vB], [PB[pvb]])
                dB = b(f"den{par}")
                for hh in range(2):
                    for cc in range(2):
                        s_ = hh * 2 + cc
                        hd = 4 * j + 2 * cc + hh
                        TT("dve", dn[:, s_:s_ + 1], pv[:, s_, 64:65], esk[:, hd:hd + 1], ALU.add, [PB[pvb], SMALL[2]], [dB])
                S_.op("dve", lambda e, dn=dn: e.reciprocal(out=dn, in_=dn), [dB], [dB])
                oB = b(f"oat{par}")
                for hh in range(2):
                    TT("dve", oat[par].rearrange("p (cc hh d) -> p hh cc d", hh=2, d=64)[:, hh], pv[:, 2 * hh:2 * hh + 2, 0:64],
                       dn[:, 2 * hh:2 * hh + 2].unsqueeze(2).to_broadcast([128, 2, 64]), ALU.mult, [PB[pvb], dB], [oB])
                for cc in range(2):
                    TR(tbv[:, cc * 128:(cc + 1) * 128], oat[par][:, cc * 128:(cc + 1) * 128], identb, [oB], [PB[tbk]])
                S_.op("act", lambda e, tb=tb, j=j, tbv=tbv: e.copy(out=oT[0][:, 2 * j:2 * j + 2, tb * 128:(tb + 1) * 128],
                                                                   in_=tbv[:, 0:256].rearrange("p (c t) -> p c t", t=128)), [PB[tbk]], [OB[0]])
            S_.barrier()
            if dbg == "swa3":
                return

    def sb_phase(l):
        win = din["w_in_p"][l]
        wa = Alloc(W_BASE)
        wsb = wa([128, 8, 3, 128], BF16)
        qT = wa([128, S], BF16)
        kT = wa([128, S], BF16)
        vtok = wa([128, NB, 128], BF16)
        spf = [wa([128, 512], F32) for _ in range(3)]
        spb = [wa([128, 512], BF16) for _ in range(3)]
        Wt = [wa([128, 512], BF16) for _ in range(3)]
        Ssum = wa([128, 512], F32)
        Ssb = [wa([128, 512], BF16) for _ in range(2)]
        rr = lambda a: a.rearrange("(kc p) n -> p kc n", p=128)
        wB = b("wsb")

        def load_w(c):
            LD("pool", wsb.rearrange("p a b c -> p (a b c)"), win[:, P_SB + c * 3072:P_SB + (c + 1) * 3072], [wB])

        load_w(0)
        zi = 0
        for c in range(4):
            qB, kB_, vB = b("qT"), b("kT"), b("vtok")
            for tg in range(NG):
                for which in range(2):
                    bk = zi % 4
                    zi += 1
                    for kc in range(8):
                        MM(bank(bk), wsb[:, kc, which, :], hT[:, kc, tg * 512:(tg + 1) * 512], kc == 0, kc == 7, [wB, HB], [PB[bk]])
                    if which == 0:
                        ACT(qT[:, tg * 512:(tg + 1) * 512], bank(bk), AF.Copy, [PB[bk]], [qB], scale=0.125)
                    else:
                        CP("dve", kT[:, tg * 512:(tg + 1) * 512], bank(bk), [PB[bk]], [kB_])
            for t4 in range(NB // 4):
                bk = zi % 4
                zi += 1
                for t_ in range(4):
                    tb = t4 * 4 + t_
                    for kc in range(8):
                        MM(psum[:, bk, t_ * 128:(t_ + 1) * 128], hT[:, kc, tb * 128:(tb + 1) * 128], wsb[:, kc, 2, :], kc == 0, kc == 7,
                           [wB, HB], [PB[bk]])
                CP("dve", vtok[:, t4 * 4:(t4 + 1) * 4, :], bank(bk).rearrange("p (t n) -> p t n", n=128), [PB[bk]], [vB])
            if c < 3:
                load_w(c + 1)
            for hh in range(2):
                p0, p1 = hh * 64, (hh + 1) * 64
                for qc in range(NG):
                    kbs = list(range(4 * qc + 3, -1, -1))
                    n = len(kbs)
                    pob = 4 + ((hh * NG + qc) % 2)
                    st = {}

                    def c0of(i):
                        k_ = kbs[i] - 4 * qc
                        return 128 * k_ if k_ > 0 else 0

                    def stage1(i):
                        kb = kbs[i]
                        bk = zi_base[0] % 4
                        zi_base[0] += 1
                        st[i] = bk
                        diag = kb >= 4 * qc
                        c0 = c0of(i)
                        sB, bB = b(f"spf{i % 3}"), b(f"spb{i % 3}")
                        MM(psum[:, bk, c0:512], kT[p0:p1, kb * 128:(kb + 1) * 128], qT[p0:p1, qc * 512 + c0:(qc + 1) * 512], True, True, [kB_, qB], [PB[bk]])
                        if diag:
                            MM(psum[:, bk, c0:512], identb, sbm[:, kb - 4 * qc, c0:512], False, True, [cB], [PB[bk]], skip=True)
                        ACT(spf[i % 3][:, c0:512], psum[:, bk, c0:512], AF.Exp, [PB[bk]], [sB])
                        ACT(spb[i % 3][:, c0:512], spf[i % 3][:, c0:512], AF.Ln, [sB], [bB], bias=1.0)

                    def stage2(i):
                        bk = st[i]
                        c0 = c0of(i)
                        bB = b(f"spb{i % 3}")
                        wtB = b(f"Wt{i % 3}")
                        MM(psum[:, bk, c0:512], Uneg, spb[i % 3][:, c0:512], False, True, [bB], [PB[bk]], skip=True)
                        if i > 0:
                            MM(psum[:, bk, c0:512], onesneg, Ssb[i % 2][:, c0:512], False, True, [b(f"Ssb{i % 2}")], [PB[bk]], skip=True)
                        if i == 0 and c0 > 0:
                            MS("pool", Wt[i % 3][:, 0:c0], 0.0, [wtB])
                        ACT(Wt[i % 3][:, c0:512], psum[:, bk, c0:512], AF.Exp, [PB[bk]], [wtB])
                        if i < n - 1:
                            c1 = c0of(i + 1)
                            if i == 0:
                                if c0 > 0:
                                    MS("pool", Ssum[:, 0:c0], 0.0, [b("Ssum")])
                                CP("dve", Ssum[:, c0:512], spb[i % 3][:, c0:512], [bB], [b("Ssum")])
                            else:
                                TT("dve", Ssum[:, c0:512], Ssum[:, c0:512], spb[i % 3][:, c0:512], ALU.add, [b("Ssum"), bB], [b("Ssum")])
                            CP("dve", Ssb[(i + 1) % 2][:, c1:512], Ssum[:, c1:512], [b("Ssum")], [b(f"Ssb{(i + 1) % 2}")])

                    def stage3(i):
                        kb = kbs[i]
                        c0 = 0 if i == 0 else c0of(i)
                        MM(psum[p0:p1, pob, c0:512], vtok[:, kb, p0:p1], Wt[i % 3][:, c0:512], i == 0, i == n - 1, [vB, b(f"Wt{i % 3}")], [PB[pob]])

                    zi_base = [zi]
                    for step in range(n + 2):
                        if step < n:
                            stage1(step)
                        if 0 <= step - 1 < n:
                            stage2(step - 1)
                        if 0 <= step - 2 < n:
                            stage3(step - 2)
                    zi = zi_base[0]
                    S_.op("act", lambda e, p0=p0, p1=p1, pob=pob, c=c, qc=qc: e.copy(
                        out=oT[1][p0:p1, c, qc * 512:(qc + 1) * 512], in_=psum[p0:p1, pob, :]), [PB[pob]], [OB[1]])
        S_.barrier()

    def merge_phase(l):
        win = din["w_in_p"][l]
        wa = Alloc(W_BASE)
        gw = [wa([128, 8, 3, 128], BF16) for _ in range(2)]
        pw = [wa([128, 4, 3, 128], BF16) for _ in range(2)]
        ow = [wa([128, D], BF16) for _ in range(2)]
        sg = [wa([128, 512], F32) for _ in range(3)]
        tt = [wa([128, 512], F32) for _ in range(2)]
        mT = [wa([128, 512], BF16) for _ in range(2)]
        rr = lambda a: a.rearrange("(kc p) n -> p kc n", p=128)

        def load_gp(m):
            wB = b(f"mwg{m % 2}")
            LD("pool", gw[m % 2].rearrange("p a b c -> p (a b c)"), win[:, P_MG + m * 3072:P_MG + (m + 1) * 3072], [wB])
            LD("pool", pw[m % 2].rearrange("p a b c -> p (a b c)"), din["w_pj_p"][l][:, m * 1536:(m + 1) * 1536], [wB])

        def load_o(m):
            LD("pool", ow[m % 2], din["w_out"][l][m * 128:(m + 1) * 128, :], [b(f"mwo{m % 2}")])

        cnt = {"gi": 0, "oi": 0}

        def GP(m, tg, sp_):
            wB = b(f"mwg{m % 2}")
            tsl = slice(tg * 512, (tg + 1) * 512)
            for i in range(3):
                bk = cnt["gi"] % 4
                cnt["gi"] += 1
                for kc in range(8):
                    MM(bank(bk), gw[m % 2][:, kc, i, :], hT[:, kc, tsl], kc == 0, kc == 7, [wB, HB], [PB[bk]])
                ACT(sg[i], bank(bk), AF.Sigmoid, [PB[bk], SMALL[11]], [b(f"sg{i}")], bias=bg[:, i * 8 + m:i * 8 + m + 1], scale=1.0)
            mB = b(f"mT{sp_ % 2}")
            for i in range(3):
                bk = cnt["gi"] % 4
                cnt["gi"] += 1
                for kc in range(4):
                    MM(bank(bk), pw[m % 2][:, kc, i, :], oT[i][:, kc, tsl], kc == 0, kc == 3, [wB, OB[i]], [PB[bk]])
                if i == 0:
                    TT("dve", tt[0], sg[0], bank(bk), ALU.mult, [b("sg0"), PB[bk]], [b("tt0")])
                elif i == 1:
                    TT("dve", tt[1], sg[1], bank(bk), ALU.mult, [b("sg1"), PB[bk]], [b("tt1")])
                    TT("pool", tt[0], tt[0], tt[1], ALU.add, [b("tt0"), b("tt1")], [b("tt0")])
                else:
                    TT("dve", tt[1], sg[2], bank(bk), ALU.mult, [b("sg2"), PB[bk]], [b("tt1")])
                    TT("pool", mT[sp_ % 2], tt[0], tt[1], ALU.add, [b("tt0"), b("tt1")], [mB])

        def OUT(m, tg, sp_):
            mB = b(f"mT{sp_ % 2}")
            for t_ in range(4):
                tb = tg * 4 + t_
                for cg in range(2):
                    bk = 4 + (cnt["oi"] % 4)
                    cnt["oi"] += 1
                    MM(bank(bk), mT[sp_ % 2][:, t_ * 128:(t_ + 1) * 128], ow[m % 2][:, cg * 512:(cg + 1) * 512], True, True,
                       [mB, b(f"mwo{m % 2}")], [PB[bk]])
                    xs = xres[:, tb, cg * 512:(cg + 1) * 512]
                    TT("dve", xs, xs, bank(bk), ALU.add, [XB[tb][cg], PB[bk]], [XB[tb][cg]])

        load_gp(0)
        load_o(0)
        prev = None
        for m in range(8):
            if m < 7:
                load_gp(m + 1)
            for tg in range(NG):
                GP(m, tg, m * NG + tg)
                if prev is not None:
                    OUT(*prev)
                if tg == 0 and m < 7:
                    load_o(m + 1)
                prev = (m, tg, m * NG + tg)
        OUT(*prev)
        S_.barrier()

    class FFN:
        def __init__(self, units, base):
            wa = Alloc(base)
            self.wg = [wa([128, 8, 512], BF16) for _ in range(2)]
            self.wu = [wa([128, 8, 512], BF16) for _ in range(2)]
            self.wd = [wa([128, 4, D], BF16) for _ in range(2)]
            self.sg = [wa([128, 512], F32) for _ in range(2)]
            self.act = [wa([128, 4, 512], BF16) for _ in range(2)]
            self.end = wa.off
            NFG = DFF // 512
            self.items = [(u, fg) for u in units for fg in range(NFG)]

        def load(self, n):
            (wg_d, wu_d, wd_d, _e), fg = self.items[n]
            rr = lambda a: a.rearrange("(kc p) n -> p kc n", p=128)
            wB = b(f"fw{n % 2}")
            LD("pool", self.wg[n % 2], rr(wg_d[:, fg * 512:(fg + 1) * 512]), [wB])
            LD("pool", self.wu[n % 2], rr(wu_d[:, fg * 512:(fg + 1) * 512]), [wB])
            LD("pool", self.wd[n % 2], rr(wd_d[fg * 512:(fg + 1) * 512, :]), [wB])

        def run(self):
            gi = oi = ai = 0
            for n, ((wg_d, wu_d, wd_d, e_idx), fg) in enumerate(self.items):
                if n + 1 < len(self.items):
                    self.load(n + 1)
                wB = b(f"fw{n % 2}")
                wg, wu, wd = self.wg[n % 2], self.wu[n % 2], self.wd[n % 2]
                for tg in range(NG):
                    tsl = slice(tg * 512, (tg + 1) * 512)
                    aB = b(f"act{ai % 2}")
                    a_ = self.act[ai % 2]
                    ai += 1
                    for fc in range(4):
                        bg_ = gi % 4
                        gi += 1
                        bu_ = gi % 4
                        gi += 1
                        sB = b(f"fsg{fc % 2}")
                        for kc in range(8):
                            MM(bank(bg_), wg[:, kc, fc * 128:(fc + 1) * 128], hT[:, kc, tsl], kc == 0, kc == 7, [wB, HB], [PB[bg_]])
                        for kc in range(8):
                            MM(bank(bu_), wu[:, kc, fc * 128:(fc + 1) * 128], hT[:, kc, tsl], kc == 0, kc == 7, [wB, HB], [PB[bu_]])
                        ACT(self.sg[fc % 2], bank(bg_), AF.Silu, [PB[bg_]], [sB])
                        TT("dve", a_[:, fc, :], self.sg[fc % 2], bank(bu_), ALU.mult, [sB, PB[bu_]], [aB])
                    for t_ in range(4):
                        tb = tg * 4 + t_
                        for cg in range(2):
                            bk = 4 + (oi % 4)
                            oi += 1
                            for fc in range(4):
                                MM(bank(bk), a_[:, fc, t_ * 128:(t_ + 1) * 128], wd[:, fc, cg * 512:(cg + 1) * 512], fc == 0, fc == 3,
                                   [aB, wB], [PB[bk]])
                            xs = xres[:, tb, cg * 512:(cg + 1) * 512]
                            if e_idx is None:
                                TT("dve", xs, xs, bank(bk), ALU.add, [XB[tb][cg], PB[bk]], [XB[tb][cg]])
                            else:
                                STT(xs, bank(bk), comb[:, tb, e_idx:e_idx + 1], xs, ALU.mult, ALU.add,
                                    [XB[tb][cg], PB[bk], b("comb")], [XB[tb][cg]])
            S_.barrier()

    oB = b("out")

    def dump(br_):
        stg = view(W_BASE, [128, 4 * S], F32)
        CP("dve", stg, oT[br_].rearrange("p c s -> p (c s)"), [OB[br_]], [b("stg")])
        S_.barrier()
        LD("sp", out[0].rearrange("(p a) d -> p (a d)", p=128)[:, 0:4 * S], stg, [b("dumped")])
        S_.barrier()

    for l in range(DEPTH):
        set_layer(l)
        load_layer_consts(l)
    for seq in range(NSEQ):
        xv = din["x"][seq].rearrange("(tb p) d -> p tb d", p=128)
        for q4 in range(0, NB, 4):
            LD("sp", xres[:, q4:q4 + 4, :], xv[:, q4:q4 + 4, :], [XB[tb][cg] for tb in range(q4, q4 + 4) for cg in range(2)])
        for l in range(DEPTH):
            set_layer(l)
            norm_phase(din["attn_norm"][l], W_BASE)
            if dbg == "hT":
                break
            lru_phase(l)
            if dbg == "lru":
                dump(2)
                break
            swa_phase(l)
            if dbg in ("swa", "swa1", "swa2", "swa3"):
                dump(0)
                break
            sb_phase(l)
            if dbg == "sb":
                dump(1)
                break
            merge_phase(l)
            if dbg == "mixer":
                break
            j = l // 2
            if l % 2 == 0:
                units = [(din["w_ffn_gate"][j], din["w_ffn_up"][j], din["w_ffn_down"][j], None)]
            else:
                units = [(din["w_exp_gate"][j, e], din["w_exp_up"][j, e], din["w_exp_down"][j, e], e) for e in range(NE)]
            ffn = FFN(units, O_BASE)
            ffn.load(0)
            norm_phase(din["ffn_norm"][l], ffn.end, router_w=(din["w_router"][j] if l % 2 else None))
            ffn.run()
        ov = out[seq].rearrange("(tb p) d -> p tb d", p=128)
        for q4 in range(0, NB, 4):
            if dbg in ("lru", "swa", "sb", "swa1", "swa2", "swa3"):
                break
            S_.dma("sp", ov[:, q4:q4 + 4, :], xres[:, q4:q4 + 4, :], [XB[tb][cg] for tb in range(q4, q4 + 4) for cg in range(2)], [oB])
        S_.barrier()
    S_.barrier()
    S_.emit()
    return nc


def pack_weights(inputs):
    w_in = np.asarray(inputs["w_in"], dtype=np.float32)
    depth = w_in.shape[0]
    tiles = []
    for c in range(4):
        tiles.append(np.r_[O_XC + c * 128:O_XC + (c + 1) * 128, O_GC + c * 128:O_GC + (c + 1) * 128])
    for j in range(2):
        tiles.append(np.r_[O_QA + j * 256:O_QA + (j + 1) * 256, O_KA + j * 64:O_KA + (j + 1) * 64, O_VA + j * 64:O_VA + (j + 1) * 64])
    for c in range(4):
        tiles.append(np.r_[O_QB + c * 128:O_QB + (c + 1) * 128, O_KB + c * 128:O_KB + (c + 1) * 128, O_VB + c * 128:O_VB + (c + 1) * 128])
    for m in range(8):
        tiles.append(np.r_[O_GT + m * 128:O_GT + (m + 1) * 128, O_GT + D + m * 128:O_GT + D + (m + 1) * 128,
                           O_GT + 2 * D + m * 128:O_GT + 2 * D + (m + 1) * 128])
    W = w_in.reshape(depth, 8, 128, INC)
    parts = [W[:, :, :, t].transpose(0, 2, 1, 3).reshape(depth, 128, -1) for t in tiles]
    w_in_p = np.ascontiguousarray(np.concatenate(parts, axis=2))
    wp = np.stack([np.asarray(inputs[k], dtype=np.float32) for k in ("w_proj_a", "w_proj_b", "w_proj_c")], axis=1)
    wp = wp.reshape(depth, 3, 4, 128, 8, 128)
    w_pj_p = np.ascontiguousarray(wp.transpose(0, 3, 4, 2, 1, 5).reshape(depth, 128, 8 * 4 * 3 * 128))
    out = {k: np.ascontiguousarray(v, dtype=np.float32) for k, v in inputs.items()
           if k not in ("x", "w_in", "w_proj_a", "w_proj_b", "w_proj_c")}
    out["w_in_p"] = w_in_p
    out["w_pj_p"] = w_pj_p
    return out


_NC_CACHE = {}


def kernel(**inputs):
    x = np.ascontiguousarray(inputs["x"], dtype=np.float32)
    BATCH = x.shape[0]
    per = BATCH // NCORES
    if "nc" not in _NC_CACHE:
        _NC_CACHE["nc"] = build(S=x.shape[1], NSEQ=per, DEPTH=inputs["w_in"].shape[0])
    nc = _NC_CACHE["nc"]
    shared = pack_weights(inputs)
    in_maps = []
    for i in range(NCORES):
        m = dict(shared)
        m["x"] = np.ascontiguousarray(x[i * per:(i + 1) * per])
        in_maps.append(m)
    res = run_bass_kernel_spmd(nc, in_maps, core_ids=list(range(NCORES)))
    return np.concatenate([np.asarray(r["out"]) for r in res.results], axis=0).astype(np.float32)
```

```python
import numpy as np
import concourse.bass as bass
import concourse.mybir as mybir
from concourse.bass_utils import run_bass_kernel_spmd

F32 = mybir.dt.float32
BF16 = mybir.dt.bfloat16
AF = mybir.ActivationFunctionType
ALU = mybir.AluOpType
AX = mybir.AxisListType

D = 1024
HD = 64
INC = 6400
DFF = 3584
NE = 8
EPS = 1e-6
O_QA, O_KA, O_VA, O_QB, O_KB, O_VB, O_XC, O_GC, O_GT = 0, 512, 640, 768, 1280, 1792, 2304, 2816, 3328
NCORES = 8
P_LRU, P_SWA, P_SB, P_MG = 0, 8192, 14336, 26624


class Buf:
    __slots__ = ("name", "w", "rs")

    def __init__(self, name):
        self.name = name
        self.w = None
        self.rs = {}


class Sched:
    SEM_LIMIT = 30000
    NDMA = 6

    def __init__(self, nc):
        self.nc = nc
        self.names = ["pe", "act", "dve", "pool", "sp"]
        self.lists = {k: [] for k in self.names}
        self.seen = {k: {} for k in self.names}
        self.cur = {}
        self.sems = {}
        self.last = {}
        self.dma_slots = {}
        self.dma_rr = {k: 0 for k in self.names}
        self.ctxs = []

    def _newsem(self, name):
        cm = self.nc.semaphore(name)
        self.sems[name] = cm.__enter__()
        self.ctxs.append(cm)
        return name

    def _eng_event(self, eng):
        c = self.cur.get(eng)
        if c is None or c[1] >= self.SEM_LIMIT:
            ep = 0 if c is None else c[2] + 1
            c = [self._newsem(f"s_{eng}_{ep}"), 0, ep]
            self.cur[eng] = c
        c[1] += 1
        self.last[c[0]] = c[1]
        return (c[0], c[1])

    def _need(self, eng, evs):
        out = {}
        for ev in evs:
            if ev is None:
                continue
            k, v = ev
            if self.seen[eng].get(k, 0) >= v:
                continue
            if out.get(k, 0) < v:
                out[k] = v
        for k, v in out.items():
            self.seen[eng][k] = v
        return list(out.items())

    def _deps(self, eng, reads, writes):
        c = self.cur.get(eng)
        own = c[0] if c else None
        evs = []
        for b in reads:
            evs.append(b.w)
        for b in writes:
            evs.append(b.w)
            evs.extend(b.rs.items())
        if eng == "pe":
            evs = [e for e in evs if e is not None and e[0] != own]
        return self._need(eng, evs)

    def _commit(self, ev, reads, writes):
        for b in reads:
            if b.rs.get(ev[0], 0) < ev[1]:
                b.rs[ev[0]] = ev[1]
        for b in writes:
            b.w = ev
            b.rs = {}

    def op(self, eng, fn, reads=(), writes=()):
        waits = self._deps(eng, reads, writes)
        ev = self._eng_event(eng)
        self.lists[eng].append((waits, fn, ev, 1))
        self._commit(ev, reads, writes)
        return ev

    def dma(self, q, out, in_, reads=(), writes=(), **kw):
        slots = self.dma_slots.setdefault(q, [])
        if len(slots) < self.NDMA:
            slots.append([self._newsem(f"d_{q}_{len(slots)}"), 0])
        slot = slots[self.dma_rr[q] % self.NDMA]
        self.dma_rr[q] += 1
        waits = self._deps(q, reads, writes)
        if slot[1] > 0:
            waits += self._need(q, [(slot[0], slot[1])])
        slot[1] += 16
        ev = (slot[0], slot[1])
        self.last[slot[0]] = slot[1]
        fn = lambda e: e.dma_start(out=out, in_=in_, **kw)
        self.lists[q].append((waits, fn, ev, 16))
        self._commit(ev, reads, writes)
        return ev

    def barrier(self):
        evs = list(self.last.items())
        for eng in self.names:
            waits = self._need(eng, evs)
            if waits:
                self.lists[eng].append((waits, None, None, 0))

    def emit(self):
        nc = self.nc
        engs = {"pe": "tensor", "act": "scalar", "dve": "vector", "pool": "gpsimd", "sp": "sync"}
        with nc.Block() as block:
            def mk(name):
                def body(e):
                    for waits, fn, ev, inc in self.lists[name]:
                        for k, v in waits:
                            e.wait_ge(self.sems[k], v)
                        if fn is not None:
                            fn(e).then_inc(self.sems[ev[0]], inc)
                return body
            for name in self.names:
                getattr(block, engs[name])(mk(name))
        for cm in reversed(self.ctxs):
            cm.__exit__(None, None, None)


def build(S=2048, NSEQ=2, DEPTH=2, dbg=None):
    NB = S // 128
    NG = S // 512
    LT = 512
    nc = bass.Bass("TRN2", target_bir_lowering=False)
    din = {}

    def dram_in(name, shape):
        din[name] = nc.dram_tensor(name, list(shape), F32, kind="ExternalInput").ap()

    NDENSE = (DEPTH + 1) // 2
    NMOE = max(DEPTH // 2, 1)
    dram_in("x", [NSEQ, S, D])
    dram_in("attn_norm", [DEPTH, D]); dram_in("w_in_p", [DEPTH, 128, 8 * INC]); dram_in("b_gate", [DEPTH, 3 * D])
    dram_in("q_norm", [DEPTH, HD]); dram_in("k_norm", [DEPTH, HD]); dram_in("sinks", [DEPTH, 8])
    dram_in("conv_w", [DEPTH, 4, 512]); dram_in("conv_b", [DEPTH, 512])
    dram_in("lru_w_r", [DEPTH, 8, 64, 64]); dram_in("lru_b_r", [DEPTH, 512])
    dram_in("lru_w_i", [DEPTH, 8, 64, 64]); dram_in("lru_b_i", [DEPTH, 512]); dram_in("lru_lambda", [DEPTH, 512])
    dram_in("w_pj_p", [DEPTH, 128, 8 * 4 * 3 * 128])
    dram_in("w_out", [DEPTH, D, D]); dram_in("ffn_norm", [DEPTH, D])
    dram_in("w_ffn_gate", [NDENSE, D, DFF]); dram_in("w_ffn_up", [NDENSE, D, DFF]); dram_in("w_ffn_down", [NDENSE, DFF, D])
    dram_in("w_router", [NMOE, D, NE]); dram_in("w_exp_gate", [NMOE, NE, D, DFF])
    dram_in("w_exp_up", [NMOE, NE, D, DFF]); dram_in("w_exp_down", [NMOE, NE, DFF, D])
    out = nc.dram_tensor("out", [NSEQ, S, D], F32, kind="ExternalOutput").ap()

    S_ = Sched(nc)
    ARENA = 208000
    arena = nc.alloc_sbuf_tensor("arena", [128, ARENA // 4], F32).ap()
    psum = nc.alloc_psum_tensor("psum", [128, 8, 512], F32).ap()

    def bank(i):
        return psum[:, i, :]

    def view(off, shape, dt):
        n = int(np.prod(shape[1:]))
        sz = 4 if dt == F32 else 2
        assert off % 4 == 0 and (n * sz) % 4 == 0, (off, shape)
        assert off + n * sz <= ARENA, ("arena overflow", off, shape)
        w = arena[:, off // 4:(off + n * sz) // 4]
        v = w if dt == F32 else w.bitcast(dt)
        if len(shape) == 3:
            v = v.rearrange("p (a b) -> p a b", b=shape[2])
        elif len(shape) == 4:
            v = v.rearrange("p (a b c) -> p a b c", b=shape[2], c=shape[3])
        return v

    class Alloc:
        def __init__(self, base):
            self.off = base

        def __call__(self, shape, dt):
            n = int(np.prod(shape[1:])) * (4 if dt == F32 else 2)
            n = (n + 31) // 32 * 32
            v = view(self.off, shape, dt)
            self.off += n
            return v

    pa = Alloc(0)
    identb = pa([128, 128], BF16)
    identf = pa([128, 128], F32)
    onesb = pa([128, 128], BF16)
    Uneg = pa([128, 128], BF16)
    onesneg = pa([128, 128], BF16)
    mD = pa([128, 512], BF16)
    mP = pa([128, 512], BF16)
    sbm = pa([128, 4, 512], BF16)
    gt = pa([128, D], F32)
    LC = []
    for _l in range(DEPTH):
        LC.append(dict(qg8=pa([128, HD], F32), kg=pa([128, HD], F32), esk=pa([128, 8], F32), cw=pa([128, 4, 4], F32),
                       cb=pa([128, 4], F32), br=pa([128, 4], F32), bi=pa([128, 4], F32), clam=pa([128, 4], F32),
                       bg=pa([128, 24], F32), wbd=pa([128, 4, 2, 128], BF16)))
    qg8 = kg = esk = cw = cb = br = bi = clam = bg = wbd = None

    def set_layer(l):
        nonlocal qg8, kg, esk, cw, cb, br, bi, clam, bg, wbd
        d_ = LC[l]
        qg8, kg, esk, cw, cb, br, bi, clam, bg, wbd = (d_["qg8"], d_["kg"], d_["esk"], d_["cw"], d_["cb"], d_["br"], d_["bi"],
                                                       d_["clam"], d_["bg"], d_["wbd"])
    comb = pa([128, NB, NE], F32)
    hprev = pa([128, 2], F32)
    xres = pa([128, NB, D], F32)
    hT = pa([128, 8, S], BF16)
    O_BASE = pa.off
    oT = [pa([128, 4, S], BF16) for _ in range(3)]
    W_BASE = pa.off

    B = {}

    def b(name):
        if name not in B:
            B[name] = Buf(name)
        return B[name]

    PB = [b(f"bank{i}") for i in range(8)]

    def MM(out, lhsT, rhs, start, stop, reads, writes, skip=False):
        S_.op("pe", lambda e: e.matmul(out=out, lhsT=lhsT, rhs=rhs, start=start, stop=stop, skip_group_check=skip), reads, writes)

    def TR(out, in_, ident, reads, writes):
        S_.op("pe", lambda e: e.transpose(out=out, in_=in_, identity=ident), reads, writes)

    def ACT(out, in_, func, reads, writes, **kw):
        S_.op("act", lambda e: e.activation(out=out, in_=in_, func=func, **kw), reads, writes)

    def TT(eng, out, in0, in1, op, reads, writes):
        S_.op(eng, lambda e: e.tensor_tensor(out=out, in0=in0, in1=in1, op=op), reads, writes)

    def TS(eng, out, in0, s1, s2, op0, op1, reads, writes):
        S_.op(eng, lambda e: e.tensor_scalar(out=out, in0=in0, scalar1=s1, scalar2=s2, op0=op0, op1=op1), reads, writes)

    def STT(out, in0, scalar, in1, op0, op1, reads, writes):
        S_.op("dve", lambda e: e.scalar_tensor_tensor(out=out, in0=in0, scalar=scalar, in1=in1, op0=op0, op1=op1), reads, writes)

    def CP(eng, out, in_, reads, writes):
        S_.op(eng, lambda e: e.tensor_copy(out=out, in_=in_), reads, writes)

    def MS(eng, ap, val, writes):
        S_.op(eng, lambda e: e.memset(ap, val), (), writes)

    def ASEL(out, in_, pattern, cmp, fill, base, cm, reads, writes):
        S_.op("pool", lambda e: e.affine_select(out=out, in_=in_, pattern=pattern, compare_op=cmp, fill=fill,
                                                base=base, channel_multiplier=cm), reads, writes)

    def LD(q, out_, in_, writes, **kw):
        S_.dma(q, out_, in_, (), writes, **kw)

    bank7b = psum[:, 7, :].bitcast(BF16)

    tmpc = view(W_BASE, [128, 512], F32)
    cB = b("consts")
    MS("pool", identf, 0.0, [cB])
    ASEL(identf, identf, [[-1, 128]], ALU.not_equal, 1.0, 0, 1, [cB], [cB])
    CP("dve", identb, identf, [cB], [cB])
    MS("pool", onesb, 1.0, [cB])
    MS("pool", onesneg, -1.0, [cB])
    MS("pool", tmpc[:, 0:128], -1.0, [cB])
    ASEL(tmpc[:, 0:128], tmpc[:, 0:128], [[-1, 128]], ALU.is_ge, 0.0, 0, 1, [cB], [cB])
    CP("dve", Uneg, tmpc[:, 0:128], [cB], [cB])
    MS("pool", tmpc, 1.0, [cB])
    ASEL(tmpc.rearrange("p (a t) -> p a t", t=128), tmpc.rearrange("p (a t) -> p a t", t=128), [[0, 4], [1, 128]],
         ALU.is_ge, 0.0, 0, -1, [cB], [cB])
    TS("dve", tmpc, tmpc, 30000.0, -30000.0, ALU.mult, ALU.add, [cB], [cB])
    CP("dve", mD, tmpc, [cB], [cB])
    MS("pool", tmpc, 1.0, [cB])
    ASEL(tmpc.rearrange("p (a t) -> p a t", t=128), tmpc.rearrange("p (a t) -> p a t", t=128), [[0, 4], [-1, 128]],
         ALU.is_gt, 0.0, 0, 1, [cB], [cB])
    TS("dve", tmpc, tmpc, 30000.0, -30000.0, ALU.mult, ALU.add, [cB], [cB])
    CP("dve", mP, tmpc, [cB], [cB])
    for k in range(4):
        MS("pool", tmpc, 1.0, [cB])
        ASEL(tmpc, tmpc, [[1, 512]], ALU.is_gt, 0.0, -128 * k, -1, [cB], [cB])
        TS("dve", tmpc, tmpc, 30000.0, -30000.0, ALU.mult, ALU.add, [cB], [cB])
        CP("dve", sbm[:, k, :], tmpc, [cB], [cB])
    S_.barrier()

    XB = [[b(f"x{tb}_{cg}") for cg in range(2)] for tb in range(NB)]
    HB = b("hT")
    OB = [b(f"oT{i}") for i in range(3)]

    def xbufs(tb):
        return XB[tb]

    SMALL = [b(n) for n in ("s_qg8", "s_kg", "s_esk", "s_cw0", "s_cw1", "s_cw2", "s_cw3", "s_cb", "s_br", "s_bi", "s_clam", "s_bg")]
    WBD = [b(f"s_wbd{i}") for i in range(16)]

    def load_layer_consts(l):
        NCD = dict(allow_slow_non_contiguous=True)
        sq_, sk_, se_, c0, c1, c2, c3, scb, sbr, sbi, scl, sbg = SMALL
        LD("sp", qg8, din["q_norm"][l].partition_broadcast(128), [sq_])
        LD("sp", kg, din["k_norm"][l].partition_broadcast(128), [sk_])
        LD("sp", esk, din["sinks"][l].partition_broadcast(128), [se_])
        for k, cB_ in enumerate((c0, c1, c2, c3)):
            LD("sp", cw[:, :, k], din["conv_w"][l, k].rearrange("(c p) -> p c", p=128), [cB_], **NCD)
        LD("sp", cb, din["conv_b"][l].rearrange("(c p) -> p c", p=128), [scb], **NCD)
        LD("sp", br, din["lru_b_r"][l].rearrange("(c p) -> p c", p=128), [sbr], **NCD)
        LD("sp", bi, din["lru_b_i"][l].rearrange("(c p) -> p c", p=128), [sbi], **NCD)
        LD("sp", clam, din["lru_lambda"][l].rearrange("(c p) -> p c", p=128), [scl], **NCD)
        LD("sp", bg, din["b_gate"][l].rearrange("(c p) -> p c", p=128), [sbg], **NCD)
        MS("pool", wbd, 0.0, WBD)
        n_ = 0
        for c in range(4):
            for hh in range(2):
                LD("pool", wbd[hh * 64:(hh + 1) * 64, c, 0, hh * 64:(hh + 1) * 64], din["lru_w_r"][l, 2 * c + hh], [WBD[n_]])
                LD("pool", wbd[hh * 64:(hh + 1) * 64, c, 1, hh * 64:(hh + 1) * 64], din["lru_w_i"][l, 2 * c + hh], [WBD[n_ + 1]])
                n_ += 2
        TS("dve", qg8, qg8, 0.125, None, ALU.mult, ALU.bypass, [sq_], [sq_])
        ACT(esk, esk, AF.Exp, [se_], [se_])
        ACT(clam, clam, AF.Exp, [scl], [scl], scale=-1.0)
        ACT(clam, clam, AF.Ln, [scl], [scl], bias=1.0)
        TS("dve", clam, clam, -8.0, None, ALU.mult, ALU.bypass, [scl], [scl])
        S_.barrier()

    def norm_phase(gain_dram, base, router_w=None):
        wa = Alloc(base)
        junk = wa([128, D], BF16)
        hb = [wa([128, D], BF16) for _ in range(2)]
        ss = wa([128, 4], F32)
        gB = b("gt")
        LD("sp", gt, gain_dram.partition_broadcast(128), [gB])
        if router_w is not None:
            hf2 = [wa([128, D], F32) for _ in range(2)]
            hTf2 = [wa([128, 8, 128], F32) for _ in range(2)]
            wr = wa([128, 8, NE], F32)
            lg2 = [wa([128, NE], F32) for _ in range(2)]
            t82 = [[wa([128, NE], F32) for _ in range(3)] for _ in range(2)]
            m122 = [wa([128, 4], F32) for _ in range(2)]
            LD("sp", wr, router_w.rearrange("(kc p) e -> p kc e", p=128), [b("wr")])
        for tb in range(NB):
            par = tb % 2
            xr = xres[:, tb, :]
            ssB, hbB = b(f"ss{par}"), b(f"hb{par}")
            ACT(junk, xr, AF.Square, xbufs(tb), [b("junk"), ssB], accum_out=ss[:, par:par + 1])
            ACT(ss[:, par:par + 1], ss[:, par:par + 1], AF.Ln, [ssB], [ssB], scale=1.0 / D, bias=EPS)
            ACT(ss[:, par:par + 1], ss[:, par:par + 1], AF.Exp, [ssB], [ssB], scale=-0.5)
            STT(hb[par], xr, ss[:, par:par + 1], gt, ALU.mult, ALU.mult, xbufs(tb) + [ssB, gB], [hbB])
            tbk = 7 if par == 0 else 3
            tbv = psum[:, tbk, :].bitcast(BF16)
            for c in range(8):
                TR(tbv[:, c * 128:(c + 1) * 128], hb[par][:, c * 128:(c + 1) * 128], identb, [hbB], [PB[tbk]])
            if tb % 2:
                CP("dve", hT[:, :, tb * 128:(tb + 1) * 128], tbv.rearrange("p (c t) -> p c t", t=128), [PB[tbk]], [HB])
            else:
                S_.op("act", lambda e, tb=tb, tbv=tbv: e.copy(out=hT[:, :, tb * 128:(tb + 1) * 128],
                                                              in_=tbv.rearrange("p (c t) -> p c t", t=128)), [PB[tbk]], [HB])
            if router_w is not None:
                hf, hTf, lg, t8, m12 = hf2[par], hTf2[par], lg2[par], t82[par], m122[par]
                tb0 = 5 if par == 0 else 1
                lbk = 4 if par == 0 else 0
                hfB, hTB, lB, mB_ = b(f"hf{par}"), b(f"hTf{par}"), b(f"lg{par}"), b(f"m12{par}")
                t0B, t1B, t2B = b(f"t80{par}"), b(f"t81{par}"), b(f"t82{par}")
                STT(hf, xr, ss[:, par:par + 1], gt, ALU.mult, ALU.mult, xbufs(tb) + [ssB, gB], [hfB])
                for c in range(8):
                    TR(psum[:, tb0 + c // 4, (c % 4) * 128:(c % 4 + 1) * 128], hf[:, c * 128:(c + 1) * 128], identf,
                       [hfB], [PB[tb0 + c // 4]])
                CP("dve", hTf, psum[:, tb0:tb0 + 2, :].rearrange("p a (c t) -> p (a c) t", t=128), [PB[tb0], PB[tb0 + 1]], [hTB])
                for c in range(8):
                    MM(psum[:, lbk, 0:NE], hTf[:, c, :], wr[:, c, :], c == 0, c == 7, [hTB, b("wr")], [PB[lbk]])
                CP("dve", lg, psum[:, lbk, 0:NE], [PB[lbk]], [lB])
                S_.op("dve", lambda e, m12=m12, lg=lg: e.reduce_max(out=m12[:, 0:1], in_=lg, axis=AX.X), [lB], [mB_])
                TS("dve", t8[0], lg, m12[:, 0:1], None, ALU.is_equal, ALU.bypass, [lB, mB_], [t0B])
                STT(t8[1], t8[0], -1e30, lg, ALU.mult, ALU.add, [t0B, lB], [t1B])
                S_.op("dve", lambda e, m12=m12, t8=t8: e.reduce_max(out=m12[:, 1:2], in_=t8[1], axis=AX.X), [t1B], [mB_])
                TS("dve", t8[0], lg, m12[:, 1:2], None, ALU.is_ge, ALU.bypass, [lB, mB_], [t0B])
                TS("dve", m12[:, 2:3], m12[:, 0:1], -1.0, None, ALU.mult, ALU.bypass, [mB_], [mB_])
                ACT(t8[1], lg, AF.Exp, [lB, mB_], [t1B], bias=m12[:, 2:3], scale=1.0)
                TT("dve", t8[2], t8[1], t8[0], ALU.mult, [t1B, t0B], [t2B])
                S_.op("dve", lambda e, m12=m12, t8=t8: e.reduce_sum(out=m12[:, 3:4], in_=t8[2], axis=AX.X), [t2B], [mB_])
                S_.op("dve", lambda e, m12=m12: e.reciprocal(out=m12[:, 3:4], in_=m12[:, 3:4]), [mB_], [mB_])
                TS("dve", comb[:, tb, :], t8[2], m12[:, 3:4], None, ALU.mult, ALU.bypass, [t2B, mB_], [b("comb")])
        S_.barrier()

    def lru_phase(l):
        wa = Alloc(W_BASE)
        xraw = wa([128, S + 4], F32)
        gg = wa([128, S], BF16)
        y2 = [wa([128, LT], F32) for _ in range(2)]
        r2 = [wa([128, LT], F32) for _ in range(2)]
        ii2 = [wa([128, LT], F32) for _ in range(2)]
        t12 = [wa([128, LT], F32) for _ in range(2)]
        xcb2 = [wa([128, LT], BF16) for _ in range(2)]
        ti_ = 0
        wl = [wa([128, 8, 2, 128], BF16) for _ in range(2)]
        win = din["w_in_p"][l]
        MS("pool", xraw[:, 0:3], 0.0, [b("xraw")])
        bi_ = 0
        def load_wl(c):
            LD("pool", wl[c % 2].rearrange("p a b c -> p (a b c)"), win[:, P_LRU + c * 2048:P_LRU + (c + 1) * 2048], [b(f"wl{c % 2}")])

        load_wl(0)
        for c in range(4):
            wB = b(f"wl{c % 2}")
            for tg in range(NG):
                for which in range(2):
                    bk = bi_ % 4
                    bi_ += 1
                    for kc in range(8):
                        MM(bank(bk), wl[c % 2][:, kc, which, :], hT[:, kc, tg * 512:(tg + 1) * 512], kc == 0, kc == 7, [wB, HB], [PB[bk]])
                    if which == 0:
                        S_.op("act", lambda e, bk=bk, tg=tg: e.copy(out=xraw[:, 3 + tg * 512:3 + (tg + 1) * 512], in_=bank(bk)), [PB[bk]], [b("xraw")])
                    else:
                        ACT(gg[:, tg * 512:(tg + 1) * 512], bank(bk), AF.Gelu_apprx_tanh, [PB[bk]], [b("gg")])
            if c < 3:
                load_wl(c + 1)
            tiles_ = {}

            def A1(half):
                nonlocal ti_, bi_
                t0 = half * LT
                pr_ = ti_ % 2
                ti_ += 1
                y, r, ii, t1, xcb = y2[pr_], r2[pr_], ii2[pr_], t12[pr_], xcb2[pr_]
                yB, rB, iB, tB, xcB = b(f"y{pr_}"), b(f"r{pr_}"), b(f"i{pr_}"), b(f"t1{pr_}"), b(f"xcb{pr_}")
                tiles_[half] = (t0, y, r, ii, t1, yB, rB, iB, tB)
                TS("dve", y, xraw[:, t0:t0 + LT], cw[:, c, 0:1], cb[:, c:c + 1], ALU.mult, ALU.add, [b("xraw")], [yB])
                for k in range(1, 4):
                    STT(y, xraw[:, t0 + k:t0 + k + LT], cw[:, c, k:k + 1], y, ALU.mult, ALU.add, [b("xraw"), yB], [yB])
                CP("pool", xcb, y, [yB], [xcB])
                for sub in range(LT // 512):
                    for which, dst, dB, bias in ((0, r, rB, br), (1, ii, iB, bi)):
                        bk = bi_ % 4
                        bi_ += 1
                        MM(bank(bk), wbd[:, c, which, :], xcb[:, sub * 512:(sub + 1) * 512], True, True, [xcB] + WBD, [PB[bk]])
                        ACT(dst[:, sub * 512:(sub + 1) * 512], bank(bk), AF.Sigmoid, [PB[bk]], [dB], bias=bias[:, c:c + 1], scale=1.0)
                ACT(r, r, AF.Exp, [rB], [rB], scale=clam[:, c:c + 1])
                TT("pool", t1, r, r, ALU.mult, [rB], [tB])
                ACT(t1, t1, AF.Sqrt, [tB], [tB], scale=-1.0, bias=1.0)
                TT("pool", ii, ii, y, ALU.mult, [iB, yB], [iB])

            def A2(half):
                t0, y, r, ii, t1, yB, rB, iB, tB = tiles_[half]
                TT("dve", t1, t1, ii, ALU.mult, [tB, iB], [tB])
                init = 0.0 if half == 0 else hprev[:, 0:1]
                S_.op("dve", lambda e, init=init, y=y, r=r, t1=t1: e.tensor_tensor_scan(out=y, data0=r, data1=t1, initial=init,
                                                                       op0=ALU.mult, op1=ALU.add),
                      [rB, tB, b("hprev")], [yB])
                CP("dve", hprev[:, 0:1], y[:, LT - 1:LT], [yB], [b("hprev")])
                TT("dve", oT[2][:, c, t0:t0 + LT], y, gg[:, t0:t0 + LT], ALU.mult, [yB, b("gg")], [OB[2]])

            nt_ = S // LT
            A1(0)
            for half in range(nt_):
                if half + 1 < nt_:
                    A1(half + 1)
                A2(half)
        S_.barrier()

    def swa_phase(l):
        win = din["w_in_p"][l]
        for j in range(2):
            wa = Alloc(W_BASE)
            wsw_all = [wa([128, 8, 384], BF16) for _ in range(2)]
            wsw = wsw_all[j]
            qkT = wa([128, 3, S], BF16)
            vaug = wa([128, NB, 66], BF16)
            sqv2 = [wa([128, 320], F32) for _ in range(2)]
            ssq2 = [wa([128, 8], F32) for _ in range(2)]
            tmpq2 = [wa([128, 320], F32) for _ in range(2)]
            qn = [wa([128, 384], BF16) for _ in range(2)]
            pex = [wa([128, 512], BF16) for _ in range(4)]
            oat = [wa([128, 256], BF16) for _ in range(2)]
            den = wa([128, 8], F32)
            wB = b(f"wsw{j}")
            rr = lambda a: a.rearrange("(kc p) n -> p kc n", p=128)
            if j == 0:
                for j2 in range(2):
                    LD("pool", wsw_all[j2].rearrange("p a b -> p (a b)"), win[:, P_SWA + j2 * 3072:P_SWA + (j2 + 1) * 3072], [b(f"wsw{j2}")])
            vB, qkB = b("vaug"), b("qkT")
            MS("pool", vaug[:, :, 64:65], 1.0, [vB])
            for tb in range(NB):
                bk = tb % 4
                par = tb % 2
                sqv, ssq, tmpq = sqv2[par], ssq2[par], tmpq2[par]
                sqB, ssB_, tqB = b(f"sqv{par}"), b(f"ssq{par}"), b(f"tmpq{par}")
                tbk = 7 if par == 0 else 6
                tbv = psum[:, tbk, :].bitcast(BF16)
                for kc in range(8):
                    MM(psum[:, bk, 0:384], hT[:, kc, tb * 128:(tb + 1) * 128], wsw[:, kc, :], kc == 0, kc == 7, [HB, wB], [PB[bk]])
                ACT(sqv, psum[:, bk, 0:320], AF.Square, [PB[bk]], [sqB])
                S_.op("dve", lambda e, sqv=sqv, ssq=ssq: e.tensor_reduce(out=ssq[:, 0:5], in_=sqv.rearrange("p (h d) -> p h d", d=64), axis=AX.X, op=ALU.add),
                      [sqB], [ssB_])
                ACT(ssq[:, 0:5], ssq[:, 0:5], AF.Ln, [ssB_], [ssB_], scale=1.0 / HD, bias=EPS)
                ACT(ssq[:, 0:5], ssq[:, 0:5], AF.Exp, [ssB_], [ssB_], scale=-0.5)
                TT("dve", tmpq.rearrange("p (h d) -> p h d", d=64), psum[:, bk, 0:320].rearrange("p (h d) -> p h d", d=64),
                   ssq[:, 0:5].unsqueeze(2).to_broadcast([128, 5, 64]), ALU.mult, [PB[bk], ssB_], [tqB])
                qnB = b(f"qn{par}")
                TT("dve", qn[par][:, 0:256].rearrange("p (h d) -> p h d", d=64), tmpq[:, 0:256].rearrange("p (h d) -> p h d", d=64),
                   qg8.unsqueeze(1).to_broadcast([128, 4, 64]), ALU.mult, [tqB, SMALL[0]], [qnB])
                TT("dve", qn[par][:, 256:384].rearrange("p (h d) -> p h d", d=64), tmpq[:, 256:320].unsqueeze(1).to_broadcast([128, 2, 64]),
                   kg.unsqueeze(1).to_broadcast([128, 2, 64]), ALU.mult, [tqB, SMALL[1]], [qnB])
                S_.op("act", lambda e, tb=tb, bk=bk: e.copy(out=vaug[:, tb, 0:64], in_=psum[:, bk, 320:384]), [PB[bk]], [vB])
                for s3 in range(3):
                    TR(tbv[:, s3 * 128:(s3 + 1) * 128], qn[par][:, s3 * 128:(s3 + 1) * 128], identb, [qnB], [PB[tbk]])
                CP("dve", qkT[:, :, tb * 128:(tb + 1) * 128], tbv[:, 0:384].rearrange("p (c t) -> p c t", t=128), [PB[tbk]], [qkB])
            pi_ = 0
            if dbg == "swa1":
                S_.barrier()
                return
            for tb in range(NB):
                kbs = [tb - 1, tb] if tb > 0 else [tb]
                pxs = []
                for kb in kbs:
                    bkp = 2 + 2 * (pi_ % 2)
                    px = pex[pi_ % 4]
                    pB = b(f"pex{pi_ % 4}")
                    pi_ += 1
                    for hh in range(2):
                        MM(psum[:, bkp + hh, 0:256].rearrange("p (a t) -> p a t", t=128), qkT[hh * 64:(hh + 1) * 64, 2, kb * 128:(kb + 1) * 128],
                           qkT[hh * 64:(hh + 1) * 64, 0:2, tb * 128:(tb + 1) * 128], True, True, [qkB], [PB[bkp + hh]])
                    for hh in range(2):
                        MM(psum[:, bkp + hh, 0:256], identb, (mD if kb == tb else mP)[:, 0:256], False, True, [cB], [PB[bkp + hh]], skip=True)
                    ACT(px.rearrange("p (a n) -> p a n", n=256), psum[:, bkp:bkp + 2, 0:256], AF.Exp, [PB[bkp], PB[bkp + 1]], [pB])
                    pxs.append((px, pB, kb))
                par = tb % 2
                if dbg == "swa2":
                    continue
                pvb = 6 if par == 0 else 0
                tbk = 7 if par == 0 else 1
                tbv = psum[:, tbk, :].bitcast(BF16)
                dn = den[:, 4 * par:4 * par + 4]
                pv = psum[:, pvb, 0:264].rearrange("p (s d) -> p s d", d=66)
                for slot in range(4):
                    for n_, (px, pB, kb) in enumerate(pxs):
                        MM(pv[:, slot, 0:65], px[:, slot * 128:(slot + 1) * 128], vaug[:, kb, 0:65], n_ == 0, n_ == len(pxs) - 1,
                           [pB, vB], [PB[pvb]])
                dB = b(f"den{par}")
                for hh in range(2):
                    for cc in range(2):
                        s_ = hh * 2 + cc
                        hd = 4 * j + 2 * cc + hh
                        TT("dve", dn[:, s_:s_ + 1], pv[:, s_, 64:65], esk[:, hd:hd + 1], ALU.add, [PB[pvb], SMALL[2]], [dB])
                S_.op("dve", lambda e, dn=dn: e.reciprocal(out=dn, in_=dn), [dB], [dB])
                oB = b(f"oat{par}")
                for hh in range(2):
                    TT("dve", oat[par].rearrange("p (cc hh d) -> p hh cc d", hh=2, d=64)[:, hh], pv[:, 2 * hh:2 * hh + 2, 0:64],
                       dn[:, 2 * hh:2 * hh + 2].unsqueeze(2).to_broadcast([128, 2, 64]), ALU.mult, [PB[pvb], dB], [oB])
                for cc in range(2):
                    TR(tbv[:, cc * 128:(cc + 1) * 128], oat[par][:, cc * 128:(cc + 1) * 128], identb, [oB], [PB[tbk]])
                S_.op("act", lambda e, tb=tb, j=j, tbv=tbv: e.copy(out=oT[0][:, 2 * j:2 * j + 2, tb * 128:(tb + 1) * 128],
                                                                   in_=tbv[:, 0:256].rearrange("p (c t) -> p c t", t=128)), [PB[tbk]], [OB[0]])
            S_.barrier()
            if dbg == "swa3":
                return

    def sb_phase(l):
        win = din["w_in_p"][l]
        wa = Alloc(W_BASE)
        wsb = wa([128, 8, 3, 128], BF16)
        qT = wa([128, S], BF16)
        kT = wa([128, S], BF16)
        vtok = wa([128, NB, 128], BF16)
        spf = [wa([128, 512], F32) for _ in range(3)]
        spb = [wa([128, 512], BF16) for _ in range(3)]
        Wt = [wa([128, 512], BF16) for _ in range(3)]
        Ssum = wa([128, 512], F32)
        Ssb = [wa([128, 512], BF16) for _ in range(2)]
        rr = lambda a: a.rearrange("(kc p) n -> p kc n", p=128)
        wB = b("wsb")

        def load_w(c):
            LD("pool", wsb.rearrange("p a b c -> p (a b c)"), win[:, P_SB + c * 3072:P_SB + (c + 1) * 3072], [wB])

        load_w(0)
        zi = 0
        for c in range(4):
            qB, kB_, vB = b("qT"), b("kT"), b("vtok")
            for tg in range(NG):
                for which in range(2):
                    bk = zi % 4
                    zi += 1
                    for kc in range(8):
                        MM(bank(bk), wsb[:, kc, which, :], hT[:, kc, tg * 512:(tg + 1) * 512], kc == 0, kc == 7, [wB, HB], [PB[bk]])
                    if which == 0:
                        ACT(qT[:, tg * 512:(tg + 1) * 512], bank(bk), AF.Copy, [PB[bk]], [qB], scale=0.125)
                    else:
                        CP("dve", kT[:, tg * 512:(tg + 1) * 512], bank(bk), [PB[bk]], [kB_])
            for t4 in range(NB // 4):
                bk = zi % 4
                zi += 1
                for t_ in range(4):
                    tb = t4 * 4 + t_
                    for kc in range(8):
                        MM(psum[:, bk, t_ * 128:(t_ + 1) * 128], hT[:, kc, tb * 128:(tb + 1) * 128], wsb[:, kc, 2, :], kc == 0, kc == 7,
                           [wB, HB], [PB[bk]])
                CP("dve", vtok[:, t4 * 4:(t4 + 1) * 4, :], bank(bk).rearrange("p (t n) -> p t n", n=128), [PB[bk]], [vB])
            if c < 3:
                load_w(c + 1)
            for hh in range(2):
                p0, p1 = hh * 64, (hh + 1) * 64
                for qc in range(NG):
                    kbs = list(range(4 * qc + 3, -1, -1))
                    n = len(kbs)
                    pob = 4 + ((hh * NG + qc) % 2)
                    st = {}

                    def c0of(i):
                        k_ = kbs[i] - 4 * qc
                        return 128 * k_ if k_ > 0 else 0

                    def stage1(i):
                        kb = kbs[i]
                        bk = zi_base[0] % 4
                        zi_base[0] += 1
                        st[i] = bk
                        diag = kb >= 4 * qc
                        c0 = c0of(i)
                        sB, bB = b(f"spf{i % 3}"), b(f"spb{i % 3}")
                        MM(psum[:, bk, c0:512], kT[p0:p1, kb * 128:(kb + 1) * 128], qT[p0:p1, qc * 512 + c0:(qc + 1) * 512], True, True, [kB_, qB], [PB[bk]])
                        if diag:
                            MM(psum[:, bk, c0:512], identb, sbm[:, kb - 4 * qc, c0:512], False, True, [cB], [PB[bk]], skip=True)
                        ACT(spf[i % 3][:, c0:512], psum[:, bk, c0:512], AF.Exp, [PB[bk]], [sB])
                        ACT(spb[i % 3][:, c0:512], spf[i % 3][:, c0:512], AF.Ln, [sB], [bB], bias=1.0)

                    def stage2(i):
                        bk = st[i]
                        c0 = c0of(i)
                        bB = b(f"spb{i % 3}")
                        wtB = b(f"Wt{i % 3}")
                        MM(psum[:, bk, c0:512], Uneg, spb[i % 3][:, c0:512], False, True, [bB], [PB[bk]], skip=True)
                        if i > 0:
                            MM(psum[:, bk, c0:512], onesneg, Ssb[i % 2][:, c0:512], False, True, [b(f"Ssb{i % 2}")], [PB[bk]], skip=True)
                        if i == 0 and c0 > 0:
                            MS("pool", Wt[i % 3][:, 0:c0], 0.0, [wtB])
                        ACT(Wt[i % 3][:, c0:512], psum[:, bk, c0:512], AF.Exp, [PB[bk]], [wtB])
                        if i < n - 1:
                            c1 = c0of(i + 1)
                            if i == 0:
                                if c0 > 0:
                                    MS("pool", Ssum[:, 0:c0], 0.0, [b("Ssum")])
                                CP("dve", Ssum[:, c0:512], spb[i % 3][:, c0:512], [bB], [b("Ssum")])
                            else:
                                TT("dve", Ssum[:, c0:512], Ssum[:, c0:512], spb[i % 3][:, c0:512], ALU.add, [b("Ssum"), bB], [b("Ssum")])
                            CP("dve", Ssb[(i + 1) % 2][:, c1:512], Ssum[:, c1:512], [b("Ssum")], [b(f"Ssb{(i + 1) % 2}")])

                    def stage3(i):
                        kb = kbs[i]
                        c0 = 0 if i == 0 else c0of(i)
                        MM(psum[p0:p1, pob, c0:512], vtok[:, kb, p0:p1], Wt[i % 3][:, c0:512], i == 0, i == n - 1, [vB, b(f"Wt{i % 3}")], [PB[pob]])

                    zi_base = [zi]
                    for step in range(n + 2):
                        if step < n:
                            stage1(step)
                        if 0 <= step - 1 < n:
                            stage2(step - 1)
                        if 0 <= step - 2 < n:
                            stage3(step - 2)
                    zi = zi_base[0]
                    S_.op("act", lambda e, p0=p0, p1=p1, pob=pob, c=c, qc=qc: e.copy(
                        out=oT[1][p0:p1, c, qc * 512:(qc + 1) * 512], in_=psum[p0:p1, pob, :]), [PB[pob]], [OB[1]])
        S_.barrier()

    def merge_phase(l):
        win = din["w_in_p"][l]
        wa = Alloc(W_BASE)
        gw = [wa([128, 8, 3, 128], BF16) for _ in range(2)]
        pw = [wa([128, 4, 3, 128], BF16) for _ in range(2)]
        ow = [wa([128, D], BF16) for _ in range(2)]
        sg = [wa([128, 512], F32) for _ in range(3)]
        tt = [wa([128, 512], F32) for _ in range(2)]
        mT = [wa([128, 512], BF16) for _ in range(2)]
        rr = lambda a: a.rearrange("(kc p) n -> p kc n", p=128)

        def load_gp(m):
            wB = b(f"mwg{m % 2}")
            LD("pool", gw[m % 2].rearrange("p a b c -> p (a b c)"), win[:, P_MG + m * 3072:P_MG + (m + 1) * 3072], [wB])
            LD("pool", pw[m % 2].rearrange("p a b c -> p (a b c)"), din["w_pj_p"][l][:, m * 1536:(m + 1) * 1536], [wB])

        def load_o(m):
            LD("pool", ow[m % 2], din["w_out"][l][m * 128:(m + 1) * 128, :], [b(f"mwo{m % 2}")])

        cnt = {"gi": 0, "oi": 0}

        def GP(m, tg, sp_):
            wB = b(f"mwg{m % 2}")
            tsl = slice(tg * 512, (tg + 1) * 512)
            for i in range(3):
                bk = cnt["gi"] % 4
                cnt["gi"] += 1
                for kc in range(8):
                    MM(bank(bk), gw[m % 2][:, kc, i, :], hT[:, kc, tsl], kc == 0, kc == 7, [wB, HB], [PB[bk]])
                ACT(sg[i], bank(bk), AF.Sigmoid, [PB[bk], SMALL[11]], [b(f"sg{i}")], bias=bg[:, i * 8 + m:i * 8 + m + 1], scale=1.0)
            mB = b(f"mT{sp_ % 2}")
            for i in range(3):
                bk = cnt["gi"] % 4
                cnt["gi"] += 1
                for kc in range(4):
                    MM(bank(bk), pw[m % 2][:, kc, i, :], oT[i][:, kc, tsl], kc == 0, kc == 3, [wB, OB[i]], [PB[bk]])
                if i == 0:
                    TT("dve", tt[0], sg[0], bank(bk), ALU.mult, [b("sg0"), PB[bk]], [b("tt0")])
                elif i == 1:
                    TT("dve", tt[1], sg[1], bank(bk), ALU.mult, [b("sg1"), PB[bk]], [b("tt1")])
                    TT("pool", tt[0], tt[0], tt[1], ALU.add, [b("tt0"), b("tt1")], [b("tt0")])
                else:
                    TT("dve", tt[1], sg[2], bank(bk), ALU.mult, [b("sg2"), PB[bk]], [b("tt1")])
                    TT("pool", mT[sp_ % 2], tt[0], tt[1], ALU.add, [b("tt0"), b("tt1")], [mB])

        def OUT(m, tg, sp_):
            mB = b(f"mT{sp_ % 2}")
            for t_ in range(4):
                tb = tg * 4 + t_
                for cg in range(2):
                    bk = 4 + (cnt["oi"] % 4)
                    cnt["oi"] += 1
                    MM(bank(bk), mT[sp_ % 2][:, t_ * 128:(t_ + 1) * 128], ow[m % 2][:, cg * 512:(cg + 1) * 512], True, True,
                       [mB, b(f"mwo{m % 2}")], [PB[bk]])
                    xs = xres[:, tb, cg * 512:(cg + 1) * 512]
                    TT("dve", xs, xs, bank(bk), ALU.add, [XB[tb][cg], PB[bk]], [XB[tb][cg]])

        load_gp(0)
        load_o(0)
        prev = None
        for m in range(8):
            if m < 7:
                load_gp(m + 1)
            for tg in range(NG):
                GP(m, tg, m * NG + tg)
                if prev is not None:
                    OUT(*prev)
                if tg == 0 and m < 7:
                    load_o(m + 1)
                prev = (m, tg, m * NG + tg)
        OUT(*prev)
        S_.barrier()

    class FFN:
        def __init__(self, units, base):
            wa = Alloc(base)
            self.wg = [wa([128, 8, 512], BF16) for _ in range(2)]
            self.wu = [wa([128, 8, 512], BF16) for _ in range(2)]
            self.wd = [wa([128, 4, D], BF16) for _ in range(2)]
            self.sg = [wa([128, 512], F32) for _ in range(2)]
            self.act = [wa([128, 4, 512], BF16) for _ in range(2)]
            self.end = wa.off
            NFG = DFF // 512
            self.items = [(u, fg) for u in units for fg in range(NFG)]

        def load(self, n):
            (wg_d, wu_d, wd_d, _e), fg = self.items[n]
            rr = lambda a: a.rearrange("(kc p) n -> p kc n", p=128)
            wB = b(f"fw{n % 2}")
            LD("pool", self.wg[n % 2], rr(wg_d[:, fg * 512:(fg + 1) * 512]), [wB])
            LD("pool", self.wu[n % 2], rr(wu_d[:, fg * 512:(fg + 1) * 512]), [wB])
            LD("pool", self.wd[n % 2], rr(wd_d[fg * 512:(fg + 1) * 512, :]), [wB])

        def run(self):
            gi = oi = ai = 0
            for n, ((wg_d, wu_d, wd_d, e_idx), fg) in enumerate(self.items):
                if n + 1 < len(self.items):
                    self.load(n + 1)
                wB = b(f"fw{n % 2}")
                wg, wu, wd = self.wg[n % 2], self.wu[n % 2], self.wd[n % 2]
                for tg in range(NG):
                    tsl = slice(tg * 512, (tg + 1) * 512)
                    aB = b(f"act{ai % 2}")
                    a_ = self.act[ai % 2]
                    ai += 1
                    for fc in range(4):
                        bg_ = gi % 4
                        gi += 1
                        bu_ = gi % 4
                        gi += 1
                        sB = b(f"fsg{fc % 2}")
                        for kc in range(8):
                            MM(bank(bg_), wg[:, kc, fc * 128:(fc + 1) * 128], hT[:, kc, tsl], kc == 0, kc == 7, [wB, HB], [PB[bg_]])
                        for kc in range(8):
                            MM(bank(bu_), wu[:, kc, fc * 128:(fc + 1) * 128], hT[:, kc, tsl], kc == 0, kc == 7, [wB, HB], [PB[bu_]])
                        ACT(self.sg[fc % 2], bank(bg_), AF.Silu, [PB[bg_]], [sB])
                        TT("dve", a_[:, fc, :], self.sg[fc % 2], bank(bu_), ALU.mult, [sB, PB[bu_]], [aB])
                    for t_ in range(4):
                        tb = tg * 4 + t_
                        for cg in range(2):
                            bk = 4 + (oi % 4)
                            oi += 1
                            for fc in range(4):
                                MM(bank(bk), a_[:, fc, t_ * 128:(t_ + 1) * 128], wd[:, fc, cg * 512:(cg + 1) * 512], fc == 0, fc == 3,
                                   [aB, wB], [PB[bk]])
                            xs = xres[:, tb, cg * 512:(cg + 1) * 512]
                            if e_idx is None:
                                TT("dve", xs, xs, bank(bk), ALU.add, [XB[tb][cg], PB[bk]], [XB[tb][cg]])
                            else:
                                STT(xs, bank(bk), comb[:, tb, e_idx:e_idx + 1], xs, ALU.mult, ALU.add,
                                    [XB[tb][cg], PB[bk], b("comb")], [XB[tb][cg]])
            S_.barrier()

    oB = b("out")

    def dump(br_):
        stg = view(W_BASE, [128, 4 * S], F32)
        CP("dve", stg, oT[br_].rearrange("p c s -> p (c s)"), [OB[br_]], [b("stg")])
        S_.barrier()
        LD("sp", out[0].rearrange("(p a) d -> p (a d)", p=128)[:, 0:4 * S], stg, [b("dumped")])
        S_.barrier()

    for l in range(DEPTH):
        set_layer(l)
        load_layer_consts(l)
    for seq in range(NSEQ):
        xv = din["x"][seq].rearrange("(tb p) d -> p tb d", p=128)
        for q4 in range(0, NB, 4):
            LD("sp", xres[:, q4:q4 + 4, :], xv[:, q4:q4 + 4, :], [XB[tb][cg] for tb in range(q4, q4 + 4) for cg in range(2)])
        for l in range(DEPTH):
            set_layer(l)
            norm_phase(din["attn_norm"][l], W_BASE)
            if dbg == "hT":
                break
            lru_phase(l)
            if dbg == "lru":
                dump(2)
                break
            swa_phase(l)
            if dbg in ("swa", "swa1", "swa2", "swa3"):
                dump(0)
                break
            sb_phase(l)
            if dbg == "sb":
                dump(1)
                break
            merge_phase(l)
            if dbg == "mixer":
                break
            j = l // 2
            if l % 2 == 0:
                units = [(din["w_ffn_gate"][j], din["w_ffn_up"][j], din["w_ffn_down"][j], None)]
            else:
                units = [(din["w_exp_gate"][j, e], din["w_exp_up"][j, e], din["w_exp_down"][j, e], e) for e in range(NE)]
            ffn = FFN(units, O_BASE)
            ffn.load(0)
            norm_phase(din["ffn_norm"][l], ffn.end, router_w=(din["w_router"][j] if l % 2 else None))
            ffn.run()
        ov = out[seq].rearrange("(tb p) d -> p tb d", p=128)
        for q4 in range(0, NB, 4):
            if dbg in ("lru", "swa", "sb", "swa1", "swa2", "swa3"):
                break
            S_.dma("sp", ov[:, q4:q4 + 4, :], xres[:, q4:q4 + 4, :], [XB[tb][cg] for tb in range(q4, q4 + 4) for cg in range(2)], [oB])
        S_.barrier()
    S_.barrier()
    S_.emit()
    return nc


def pack_weights(inputs):
    w_in = np.asarray(inputs["w_in"], dtype=np.float32)
    depth = w_in.shape[0]
    tiles = []
    for c in range(4):
        tiles.append(np.r_[O_XC + c * 128:O_XC + (c + 1) * 128, O_GC + c * 128:O_GC + (c + 1) * 128])
    for j in range(2):
        tiles.append(np.r_[O_QA + j * 256:O_QA + (j + 1) * 256, O_KA + j * 64:O_KA + (j + 1) * 64, O_VA + j * 64:O_VA + (j + 1) * 64])
    for c in range(4):
        tiles.append(np.r_[O_QB + c * 128:O_QB + (c + 1) * 128, O_KB + c * 128:O_KB + (c + 1) * 128, O_VB + c * 128:O_VB + (c + 1) * 128])
    for m in range(8):
        tiles.append(np.r_[O_GT + m * 128:O_GT + (m + 1) * 128, O_GT + D + m * 128:O_GT + D + (m + 1) * 128,
                           O_GT + 2 * D + m * 128:O_GT + 2 * D + (m + 1) * 128])
    W = w_in.reshape(depth, 8, 128, INC)
    parts = [W[:, :, :, t].transpose(0, 2, 1, 3).reshape(depth, 128, -1) for t in tiles]
    w_in_p = np.ascontiguousarray(np.concatenate(parts, axis=2))
    wp = np.stack([np.asarray(inputs[k], dtype=np.float32) for k in ("w_proj_a", "w_proj_b", "w_proj_c")], axis=1)
    wp = wp.reshape(depth, 3, 4, 128, 8, 128)
    w_pj_p = np.ascontiguousarray(wp.transpose(0, 3, 4, 2, 1, 5).reshape(depth, 128, 8 * 4 * 3 * 128))
    out = {k: np.ascontiguousarray(v, dtype=np.float32) for k, v in inputs.items()
           if k not in ("x", "w_in", "w_proj_a", "w_proj_b", "w_proj_c")}
    out["w_in_p"] = w_in_p
    out["w_pj_p"] = w_pj_p
    return out


_NC_CACHE = {}


def kernel(**inputs):
    x = np.ascontiguousarray(inputs["x"], dtype=np.float32)
    BATCH = x.shape[0]
    per = BATCH // NCORES
    if "nc" not in _NC_CACHE:
        _NC_CACHE["nc"] = build(S=x.shape[1], NSEQ=per, DEPTH=inputs["w_in"].shape[0])
    nc = _NC_CACHE["nc"]
    shared = pack_weights(inputs)
    in_maps = []
    for i in range(NCORES):
        m = dict(shared)
        m["x"] = np.ascontiguousarray(x[i * per:(i + 1) * per])
        in_maps.append(m)
    res = run_bass_kernel_spmd(nc, in_maps, core_ids=list(range(NCORES)))
    return np.concatenate([np.asarray(r["out"]) for r in res.results], axis=0).astype(np.float32)
```
